# Optimizing a Trainium2 kernel written in Bass

```python
import jax
import jax.numpy as jnp
from jax import lax
import numpy as np

D_MODEL = 1024
BATCH = 2
SEQ = 16384
DEPTH = 2

GRID_W = 64
HEAD_DIM = 64
EPS = 1e-6

NA_HEADS = 6
NA_WIN_H = 8
NA_WIN_W = 16
A_WIDTH = NA_HEADS * HEAD_DIM

MLA_HEADS = 6
MLA_Q_RANK = 256
MLA_KV_RANK = 128
MLA_NOPE = 64
MLA_ROPE = 32
MLA_QK = MLA_NOPE + MLA_ROPE
MLA_V = 64
B_WIDTH = MLA_HEADS * MLA_V
ROPE_THETA = 10000.0
Q_BLOCK = 128

CONV_GROUPS = 4
CONV_CH = CONV_GROUPS * HEAD_DIM
CONV_W = 3
C_WIDTH = CONV_CH

D_MIX = A_WIDTH + B_WIDTH + C_WIDTH
IN_SIZES = (A_WIDTH, A_WIDTH, A_WIDTH, MLA_Q_RANK, MLA_KV_RANK, MLA_ROPE, C_WIDTH, C_WIDTH, C_WIDTH)
IN_COLS = sum(IN_SIZES)

N_EXPERTS = 16
EXPERT_FF = 1024
CAPACITY_FACTOR = 2

kernel_name = 'hybrid_natten_mla_shortconv_ec_moe'


def rms_norm(x, g):
    xf = x.astype(jnp.float32)
    y = xf * lax.rsqrt(jnp.mean(xf * xf, axis=-1, keepdims=True) + EPS)
    return (y * g.astype(jnp.float32)).astype(x.dtype)


def rope(x, pos):
    half = x.shape[-1] // 2
    inv = ROPE_THETA ** (-jnp.arange(half, dtype=jnp.float32) / half)
    ang = pos.astype(jnp.float32)[:, None] * inv[None, :]
    cos = jnp.cos(ang)[None, :, None, :]
    sin = jnp.sin(ang)[None, :, None, :]
    xf = x.astype(jnp.float32)
    x1, x2 = xf[..., :half], xf[..., half:]
    return jnp.concatenate([x1 * cos - x2 * sin, x2 * cos + x1 * sin], axis=-1).astype(x.dtype)


def neighbourhood_attention(q, k, v, rpb):
    b, s, h, dh = q.shape
    rows = s // GRID_W
    wh = min(NA_WIN_H, rows)
    ww = NA_WIN_W
    cols = np.arange(GRID_W)
    col_idx = np.clip(cols - ww // 2, 0, GRID_W - ww)[:, None] + np.arange(ww)[None, :]
    col_off = col_idx - cols[:, None] + (NA_WIN_W - 1)
    qg = q.reshape(b, rows, GRID_W, h, dh).transpose(1, 0, 2, 3, 4)
    kg = k.reshape(b, rows, GRID_W, h, dh)
    vg = v.reshape(b, rows, GRID_W, h, dh)
    scale = dh ** -0.5

    def one_row(args):
        r, q_row = args
        r0 = jnp.clip(r - wh // 2, 0, rows - wh)
        k_win = lax.dynamic_slice_in_dim(kg, r0, wh, axis=1)[:, :, col_idx]
        v_win = lax.dynamic_slice_in_dim(vg, r0, wh, axis=1)[:, :, col_idx]
        row_off = r0 + jnp.arange(wh) - r + (NA_WIN_H - 1)
        bias = rpb[:, row_off[:, None, None], col_off[None, :, :]].transpose(0, 2, 1, 3)
        sc = jnp.einsum('bqhd,biqjhd->bhqij', q_row, k_win).astype(jnp.float32) * scale
        sc = sc + bias.astype(jnp.float32)[None]
        p = jax.nn.softmax(sc.reshape(b, h, GRID_W, wh * ww), axis=-1).reshape(sc.shape).astype(v.dtype)
        return jnp.einsum('bhqij,biqjhd->bqhd', p, v_win)

    o = lax.map(one_row, (jnp.arange(rows), qg))
    return o.transpose(1, 0, 2, 3, 4).reshape(b, s, h * dh)


def mla_attention(q, k, v):
    b, s, h, dq = q.shape
    dv = v.shape[-1]
    nblk = s // Q_BLOCK
    scale = dq ** -0.5
    qb = q.reshape(b, nblk, Q_BLOCK, h, dq).transpose(1, 0, 2, 3, 4)

    def one_block(q_blk):
        sc = jnp.einsum('bqhd,bkhd->bhqk', q_blk, k).astype(jnp.float32) * scale
        p = jax.nn.softmax(sc, axis=-1).astype(v.dtype)
        return jnp.einsum('bhqk,bkhd->bqhd', p, v)

    o = lax.map(one_block, qb)
    return o.transpose(1, 0, 2, 3, 4).reshape(b, s, h * dv)


def short_conv_mixer(hc, bc, cc, conv_w):
    u = cc * hc
    y = lax.conv_general_dilated(
        u, conv_w[:, None, :].astype(u.dtype), window_strides=(1,),
        padding=((CONV_W // 2, CONV_W // 2),), dimension_numbers=('NWC', 'WIO', 'NWC'),
        feature_group_count=u.shape[-1])
    return bc * y


def expert_choice_moe(x, w_router, b_router, w_gate, w_up, w_down):
    b, s, d = x.shape
    cap = CAPACITY_FACTOR * s // N_EXPERTS
    logits = jnp.einsum('bsd,de->bse', x, w_router).astype(jnp.float32) + b_router.astype(jnp.float32)
    aff = jax.nn.softmax(logits, axis=-1)
    gate, idx = lax.top_k(aff.transpose(0, 2, 1), cap)
    bidx = jnp.arange(b)[:, None, None]
    xs = x[bidx, idx]
    hid = jax.nn.silu(jnp.einsum('becd,edf->becf', xs, w_gate)) * jnp.einsum('becd,edf->becf', xs, w_up)
    ys = jnp.einsum('becf,efd->becd', hid, w_down) * gate[..., None].astype(x.dtype)
    return jnp.zeros_like(x).at[bidx, idx].add(ys)


def hybrid_layer(x, pos, norm_mix, w_in, q_norm_a, k_norm_a, rpb, cq_norm, w_uq, ckv_norm, w_ukv,
                 q_norm_b, k_norm_b, conv_w, out_norm, w_out, norm_ffn, w_router, b_router,
                 w_gate, w_up, w_down):
    b, s, _ = x.shape
    hn = rms_norm(x, norm_mix)
    proj = jnp.einsum('bsd,dc->bsc', hn, w_in)
    splits = np.cumsum(IN_SIZES)[:-1].tolist()
    qa, ka, va, cq, ckv, kr, hc, bc, cc = jnp.split(proj, splits, axis=-1)

    qa = rms_norm(qa.reshape(b, s, NA_HEADS, HEAD_DIM), q_norm_a)
    ka = rms_norm(ka.reshape(b, s, NA_HEADS, HEAD_DIM), k_norm_a)
    va = va.reshape(b, s, NA_HEADS, HEAD_DIM)
    out_a = neighbourhood_attention(qa, ka, va, rpb)

    qb = jnp.einsum('bsr,rc->bsc', rms_norm(cq, cq_norm), w_uq).reshape(b, s, MLA_HEADS, MLA_QK)
    kv = jnp.einsum('bsr,rc->bsc', rms_norm(ckv, ckv_norm), w_ukv).reshape(b, s, MLA_HEADS, MLA_NOPE + MLA_V)
    kb = jnp.concatenate(
        [kv[..., :MLA_NOPE], jnp.broadcast_to(kr[:, :, None, :], (b, s, MLA_HEADS, MLA_ROPE))], axis=-1)
    vb = kv[..., MLA_NOPE:]
    qb = rms_norm(qb, q_norm_b)
    kb = rms_norm(kb, k_norm_b)
    qb = jnp.concatenate([qb[..., :MLA_NOPE], rope(qb[..., MLA_NOPE:], pos)], axis=-1)
    kb = jnp.concatenate([kb[..., :MLA_NOPE], rope(kb[..., MLA_NOPE:], pos)], axis=-1)
    out_b = mla_attention(qb, kb, vb)

    out_c = short_conv_mixer(hc, bc, cc, conv_w)

    mixed = jnp.concatenate([
        rms_norm(out_a, out_norm[:A_WIDTH]),
        rms_norm(out_b, out_norm[A_WIDTH:A_WIDTH + B_WIDTH]),
        rms_norm(out_c, out_norm[A_WIDTH + B_WIDTH:]),
    ], axis=-1)
    x = x + jnp.einsum('bsc,cd->bsd', mixed, w_out)
    x = x + expert_choice_moe(rms_norm(x, norm_ffn), w_router, b_router, w_gate, w_up, w_down)
    return x


def setup_inputs(seed: int = 0) -> dict:
    key = jax.random.key(seed)
    ks = jax.random.split(key, 22)
    f32 = jnp.float32

    def nrm(k, shape, scale):
        return jax.random.normal(k, shape, f32) * scale

    def gain(k, shape):
        return 1.0 + 0.05 * jax.random.normal(k, shape, f32)

    res_scale = (2 * DEPTH) ** -0.5
    return {
        'x': nrm(ks[0], (BATCH, SEQ, D_MODEL), 1.0),
        'norm_mix': gain(ks[1], (DEPTH, D_MODEL)),
        'w_in': nrm(ks[2], (DEPTH, D_MODEL, IN_COLS), D_MODEL ** -0.5),
        'q_norm_a': gain(ks[3], (DEPTH, HEAD_DIM)),
        'k_norm_a': gain(ks[4], (DEPTH, HEAD_DIM)),
        'rpb': nrm(ks[5], (DEPTH, NA_HEADS, 2 * NA_WIN_H - 1, 2 * NA_WIN_W - 1), 0.1),
        'cq_norm': gain(ks[6], (DEPTH, MLA_Q_RANK)),
        'w_uq': nrm(ks[7], (DEPTH, MLA_Q_RANK, MLA_HEADS * MLA_QK), MLA_Q_RANK ** -0.5),
        'ckv_norm': gain(ks[8], (DEPTH, MLA_KV_RANK)),
        'w_ukv': nrm(ks[9], (DEPTH, MLA_KV_RANK, MLA_HEADS * (MLA_NOPE + MLA_V)), MLA_KV_RANK ** -0.5),
        'q_norm_b': gain(ks[10], (DEPTH, MLA_QK)),
        'k_norm_b': gain(ks[11], (DEPTH, MLA_QK)),
        'conv_w': nrm(ks[12], (DEPTH, CONV_W, CONV_CH), CONV_W ** -0.5),
        'out_norm': gain(ks[13], (DEPTH, D_MIX)),
        'w_out': nrm(ks[14], (DEPTH, D_MIX, D_MODEL), D_MIX ** -0.5 * res_scale),
        'norm_ffn': gain(ks[15], (DEPTH, D_MODEL)),
        'w_router': nrm(ks[16], (DEPTH, D_MODEL, N_EXPERTS), D_MODEL ** -0.5),
        'b_router': nrm(ks[17], (DEPTH, N_EXPERTS), 0.01),
        'w_gate': nrm(ks[18], (DEPTH, N_EXPERTS, D_MODEL, EXPERT_FF), D_MODEL ** -0.5),
        'w_up': nrm(ks[19], (DEPTH, N_EXPERTS, D_MODEL, EXPERT_FF), D_MODEL ** -0.5),
        'w_down': nrm(ks[20], (DEPTH, N_EXPERTS, EXPERT_FF, D_MODEL), EXPERT_FF ** -0.5),
    }


def reference(x, norm_mix, w_in, q_norm_a, k_norm_a, rpb, cq_norm, w_uq, ckv_norm, w_ukv,
              q_norm_b, k_norm_b, conv_w, out_norm, w_out, norm_ffn, w_router, b_router,
              w_gate, w_up, w_down):
    pos = jnp.arange(x.shape[1], dtype=jnp.int32)
    for l in range(DEPTH):
        x = hybrid_layer(x, pos, norm_mix[l], w_in[l], q_norm_a[l], k_norm_a[l], rpb[l], cq_norm[l],
                         w_uq[l], ckv_norm[l], w_ukv[l], q_norm_b[l], k_norm_b[l], conv_w[l],
                         out_norm[l], w_out[l], norm_ffn[l], w_router[l], b_router[l],
                         w_gate[l], w_up[l], w_down[l])
    return x
```

```python
import contextlib
import numpy as np
import ml_dtypes
import concourse.bass as bass
import concourse.mybir as mybir
from concourse.bass_utils import run_bass_kernel_spmd

F32 = mybir.dt.float32
BF16 = mybir.dt.bfloat16
AF = mybir.ActivationFunctionType
ALU = mybir.AluOpType
AX = mybir.AxisListType

NCORES = 8
D = 1024
S_LEN = 16384
TOK = 4096
NEXP = 16
CAP = 2048
EPS = 1e-6


class Buf:
    __slots__ = ("name", "w", "r")

    def __init__(self, name=""):
        self.name = name
        self.w = None
        self.r = {}


class Tl:
    def __init__(self, nc, name, shape, dtype, psum=False):
        if psum:
            self.t = nc.alloc_psum_tensor(name, shape, dtype)
        else:
            self.t = nc.alloc_sbuf_tensor(name, shape, dtype)
        self.b = Buf(name)

    def __getitem__(self, idx):
        return self.t[idx]


def _b(x):
    return x.b if hasattr(x, "b") else x


class Sched:
    ENGS = ("pe", "act", "dve", "pool", "sp")

    def __init__(self, nc):
        self.nc = nc
        self.ops = {e: [] for e in self.ENGS}
        self.cnt = {e: 0 for e in self.ENGS}
        self.seen = {e: {} for e in self.ENGS}
        self.sem_names = list(self.ENGS)

    def _dep(self, eng, reads, writes):
        waits = {}

        def need(k, v):
            if waits.get(k, 0) < v:
                waits[k] = v
        for b in reads:
            b = _b(b)
            if b.w is not None:
                need(*b.w)
        for b in writes:
            b = _b(b)
            if b.w is not None:
                need(*b.w)
            for k, v in b.r.items():
                need(k, v)
        out = []
        seen = self.seen[eng]
        for k, v in waits.items():
            if seen.get(k, 0) >= v:
                continue
            seen[k] = v
            out.append((k, v))
        return out

    def _mark(self, tok, reads, writes):
        k, v = tok
        for b in reads:
            _b(b).r[k] = v
        for b in writes:
            b = _b(b)
            b.w = tok
            b.r = {}

    def op(self, eng, name, *args, reads=(), writes=(), **kw):
        fn = (name, args, kw)
        waits = self._dep(eng, reads, writes)
        self.cnt[eng] += 1
        tok = (eng, self.cnt[eng])
        self.ops[eng].append((waits, fn, (eng, 1)))
        self._mark(tok, reads, writes)

    def dma(self, queue, out, in_, reads=(), writes=(), stream="d0"):
        fn = ("dma_start", (), {"out": out, "in_": in_})
        key = "dma_" + stream
        if key not in self.cnt:
            self.cnt[key] = 0
            self.sem_names.append(key)
        waits = self._dep(queue, reads, writes)
        self.cnt[key] += 16
        tok = (key, self.cnt[key])
        self.ops[queue].append((waits, fn, (key, 16)))
        self._mark(tok, reads, writes)

    def barrier(self, engs=None):
        for e in (engs or self.ENGS):
            waits = []
            for k, v in self.cnt.items():
                if v > 0 and self.seen[e].get(k, 0) < v:
                    self.seen[e][k] = v
                    waits.append((k, v))
            self.ops[e].append((waits, None, None))

    def finish(self):
        nc = self.nc
        self.barrier()
        with contextlib.ExitStack() as st:
            sems = {n: st.enter_context(nc.semaphore(n)) for n in self.sem_names}
            block = st.enter_context(nc.Block())

            def emit(engname):
                def f(e):
                    for waits, fn, inc in self.ops[engname]:
                        for k, v in waits:
                            e.wait_ge(sems[k], v)
                        if fn is not None:
                            getattr(e, fn[0])(*fn[1], **fn[2]).then_inc(sems[inc[0]], inc[1])
                return f
            block.tensor(emit("pe"))
            block.scalar(emit("act"))
            block.vector(emit("dve"))
            block.gpsimd(emit("pool"))
            block.sync(emit("sp"))


NPASS = 4
PTOK = TOK // NPASS
NBIS = 34


def build_moe():
    nc = bass.Bass("TRN2", target_bir_lowering=False)
    affT_d = nc.dram_tensor("aff_full", [128, 128 * NEXP], F32, kind="ExternalInput").ap()
    affo_d = nc.dram_tensor("aff_own", [128, 32 * NEXP], F32, kind="ExternalInput").ap()
    hn2_d = nc.dram_tensor("hn2T", [128, 8, TOK], BF16, kind="ExternalInput").ap()
    xmid_d = nc.dram_tensor("xmid", [128, 32, D], F32, kind="ExternalInput").ap()
    wg_d = nc.dram_tensor("wg", [NEXP, D, D], F32, kind="ExternalInput").ap()
    wu_d = nc.dram_tensor("wu", [NEXP, D, D], F32, kind="ExternalInput").ap()
    wd_d = nc.dram_tensor("wd", [NEXP, D, D], F32, kind="ExternalInput").ap()
    xout_d = nc.dram_tensor("xout", [128, 32, D], F32, kind="ExternalOutput").ap()
    S = Sched(nc)

    affT = Tl(nc, "affT", [128, 128, NEXP], F32)
    cmp_ = Tl(nc, "cmp", [128, 128, NEXP], F32)
    affo = Tl(nc, "affo", [128, 32, NEXP], F32)
    cw = Tl(nc, "cw", [128, 32, NEXP], F32)
    ones_f = Tl(nc, "ones_f", [128, 128], F32)
    small = {n: Tl(nc, n, [128, NEXP], F32) for n in ("lo", "hi", "mid", "sel", "t1", "t2", "cntp")}
    cnt_ps = Tl(nc, "cnt_ps", [128, 512], F32, psum=True)

    S.dma("sp", out=affT[:].rearrange("p j e -> p (j e)"), in_=affT_d, writes=[affT], stream="ld")
    S.dma("sp", out=affo[:].rearrange("p j e -> p (j e)"), in_=affo_d, writes=[affo], stream="ld")
    S.op("dve", "memset", ones_f[:], 1.0, writes=[ones_f])
    S.op("dve", "memset", small["lo"][:], 0.0, writes=[small["lo"]])
    S.op("dve", "memset", small["hi"][:], 1.0, writes=[small["hi"]])
    S.op("dve", "memset", small["mid"][:], 0.5, writes=[small["mid"]])
    lo, hi, mid, sel, t1, t2, cntp = (small[n] for n in ("lo", "hi", "mid", "sel", "t1", "t2", "cntp"))
    for it in range(NBIS):
        S.op("dve", "tensor_tensor", out=cmp_[:], in0=affT[:],
                                              in1=mid[:].unsqueeze(1).to_broadcast([128, 128, NEXP]), op=ALU.is_ge,
             reads=[affT, mid], writes=[cmp_])
        S.op("dve", "tensor_reduce", out=cntp[:], in_=cmp_[:].rearrange("p j e -> p e j"), axis=AX.X, op=ALU.add,
             reads=[cmp_], writes=[cntp])
        S.op("pe", "matmul", cnt_ps[:, 0:NEXP], ones_f[:], cntp[:], start=True, stop=True,
             reads=[ones_f, cntp], writes=[cnt_ps])
        S.op("dve", "tensor_scalar", out=sel[:], in0=cnt_ps[:, 0:NEXP], scalar1=float(CAP) - 0.5, scalar2=None, op0=ALU.is_ge,
             reads=[cnt_ps], writes=[sel])
        S.op("dve", "tensor_tensor", out=t1[:], in0=sel[:], in1=mid[:], op=ALU.mult, reads=[sel, mid], writes=[t1])
        S.op("dve", "tensor_tensor", out=lo[:], in0=lo[:], in1=t1[:], op=ALU.max, reads=[lo, t1], writes=[lo])
        S.op("dve", "scalar_tensor_tensor", out=t2[:], in0=sel[:], scalar=4.0, in1=mid[:], op0=ALU.mult, op1=ALU.add,
             reads=[sel, mid], writes=[t2])
        S.op("dve", "tensor_tensor", out=hi[:], in0=hi[:], in1=t2[:], op=ALU.min, reads=[hi, t2], writes=[hi])
        S.op("dve", "tensor_tensor", out=t1[:], in0=lo[:], in1=hi[:], op=ALU.add, reads=[lo, hi], writes=[t1])
        S.op("dve", "tensor_scalar", out=mid[:], in0=t1[:], scalar1=0.5, scalar2=None, op0=ALU.mult, reads=[t1], writes=[mid])
    S.op("dve", "tensor_tensor", out=cw[:], in0=affo[:], in1=lo[:].unsqueeze(1).to_broadcast([128, 32, NEXP]), op=ALU.is_ge,
         reads=[affo, lo], writes=[cw])
    S.op("dve", "tensor_tensor", out=cw[:], in0=cw[:], in1=affo[:], op=ALU.mult, reads=[cw, affo], writes=[cw])

    NW = 4
    wring = [Tl(nc, f"w{i}", [128, 8, D], BF16) for i in range(NW)]
    hn2 = [Tl(nc, f"hn2_{i}", [128, 8, PTOK], BF16) for i in range(2)]
    acc = [Tl(nc, f"acc{i}", [128, PTOK // 128, D], F32) for i in range(1)]
    H = Tl(nc, "H", [128, 8, PTOK], BF16)
    sgt = [Tl(nc, f"sg{i}", [128, 512], F32) for i in range(2)]
    pg = [Tl(nc, f"pg{i}", [128, 512], F32, psum=True) for i in range(2)]
    pu = [Tl(nc, f"pu{i}", [128, 512], F32, psum=True) for i in range(2)]
    py = [Tl(nc, f"py{i}", [128, 512], F32, psum=True) for i in range(2)]

    wseq = []
    for ps in range(NPASS):
        for ex in range(NEXP):
            wseq += [(wg_d, ex), (wu_d, ex), (wd_d, ex)]
    wstate = {"next": 0}

    def issue_w():
        i = wstate["next"]
        if i >= len(wseq):
            return
        wstate["next"] += 1
        src, ex = wseq[i]
        dst = wring[i % NW]
        S.dma("pool", out=dst[:], in_=src[ex].rearrange("(c p) f -> p c f", p=128),
              writes=[dst], stream="w")

    for _ in range(NW - 1):
        issue_w()
    wi = 0
    for ps in range(NPASS):
        hb = hn2[ps % 2]
        ac = acc[0]
        S.dma("sp", out=hb[:], in_=hn2_d[:, :, ps * PTOK:(ps + 1) * PTOK], writes=[hb], stream="ld")
        S.dma("sp", out=ac[:], in_=xmid_d[:, ps * 8:(ps + 1) * 8, :], writes=[ac], stream="ld")
        for ex in range(NEXP):
            G = wring[wi % NW]
            U = wring[(wi + 1) % NW]
            Dn = wring[(wi + 2) % NW]
            wi += 3
            issue_w()
            for blk in range(PTOK // 512):
                ts = slice(blk * 512, (blk + 1) * 512)
                for f in range(8):
                    g_, u_, s_ = pg[f % 2], pu[f % 2], sgt[f % 2]
                    fs = slice(f * 128, (f + 1) * 128)
                    for k in range(8):
                        S.op("pe", "matmul", g_[:], G[:, k, fs], hb[:, k, ts], start=(k == 0), stop=(k == 7),
                             reads=[G, hb], writes=[g_])
                    for k in range(8):
                        S.op("pe", "matmul", u_[:], U[:, k, fs], hb[:, k, ts], start=(k == 0), stop=(k == 7),
                             reads=[U, hb], writes=[u_])
                    S.op("act", "activation", out=s_[:], in_=g_[:], func=AF.Silu, reads=[g_], writes=[s_])
                    S.op("dve", "tensor_tensor", out=H[:, f, ts], in0=s_[:], in1=u_[:], op=ALU.mult,
                         reads=[s_, u_], writes=[H])
            issue_w()
            issue_w()
            for t in range(PTOK // 128):
                for dh in range(2):
                    y_ = py[(t * 2 + dh) % 2]
                    ds_ = slice(dh * 512, (dh + 1) * 512)
                    for f in range(8):
                        S.op("pe", "matmul", y_[:], H[:, f, t * 128:(t + 1) * 128], Dn[:, f, ds_], start=(f == 0), stop=(f == 7),
                             reads=[H, Dn], writes=[y_])
                    tg = ps * 8 + t
                    S.op("dve", "scalar_tensor_tensor", out=ac[:, t, ds_], in0=y_[:], scalar=cw[:, tg, ex:ex + 1],
                                                                 in1=ac[:, t, ds_], op0=ALU.mult, op1=ALU.add,
                         reads=[y_, cw, ac], writes=[ac])
        S.dma("sp", out=xout_d[:, ps * 8:(ps + 1) * 8, :], in_=ac[:], reads=[ac], stream="st")
    S.finish()
    return nc


EXT = TOK + 1024
NQB = TOK // 512
C_QA, C_KA, C_VA, C_CQ, C_CKV, C_KR, C_HC, C_BC, C_CC = 0, 384, 768, 1152, 1408, 1536, 1568, 1824, 2080


_UID = [0]


def _uq(name):
    _UID[0] += 1
    return f"{name}_u{_UID[0]}"


class Scope:
    def __init__(self, nc):
        self.nc = nc
        self.st = contextlib.ExitStack()

    def sb(self, name, shape, dtype):
        t = Tl.__new__(Tl)
        t.t = self.st.enter_context(self.nc.sbuf_tensor(_uq(name), shape, dtype))
        t.b = Buf(name)
        return t

    def ps(self, name, shape=(128, 512), dtype=F32):
        t = Tl.__new__(Tl)
        t.t = self.st.enter_context(self.nc.psum_tensor(_uq(name), list(shape), dtype))
        t.b = Buf(name)
        return t

    def close(self):
        self.st.close()


def build_mix(debug=False):
    nc = bass.Bass("TRN2", target_bir_lowering=False)

    def din(name, shape, dt=F32):
        return nc.dram_tensor(name, list(shape), dt, kind="ExternalInput").ap()

    def dscr(name, shape, dt, out=False):
        return nc.dram_tensor(name, list(shape), dt, kind="ExternalOutput" if (out or debug) else "Internal").ap()

    xTf_d = din("xT_full", [128, 8, S_LEN])
    xTe_d = din("xT_ext", [128, 8, EXT])
    w_in_d = din("w_in", [D, 2336])
    w_uq_d = din("w_uq", [256, 576])
    w_ukv_d = din("w_ukv", [128, 768])
    w_out_d = din("w_out", [D, D])
    w_rt_d = din("w_router", [D, NEXP])
    g_mix_d = din("g_mix", [128, 8])
    g_qa_d = din("g_qa", [128, 1])
    g_ka_d = din("g_ka", [128, 1])
    g_cq_d = din("g_cq", [128, 2])
    g_ckv_d = din("g_ckv", [128, 1])
    g_qb_d = din("g_qb", [96, 1])
    g_kb_d = din("g_kb", [96, 1])
    convw_d = din("conv_w", [128, 2, 3])
    g_oa_d = din("g_oa", [64, 6])
    g_ob_d = din("g_ob", [64, 6])
    g_oc_d = din("g_oc", [128, 2])
    g_ffn_d = din("g_ffn", [128, 8])
    b_rt_d = din("b_router", [NEXP, 1])
    ropeKC_d = din("ropeKC", [96, S_LEN])
    ropeKS_d = din("ropeKS", [96, S_LEN])
    ropeQC_d = din("ropeQC", [96, TOK])
    ropeQS_d = din("ropeQS", [96, TOK])
    rot_d = din("rotT", [96, 96])
    bd_d = din("blockdiag", [128, 128])
    ident_d = din("ident", [128, 128])
    biasA_d = din("biasA", [3, 6, 128, 8, 512])

    xmid_o = dscr("xmidT", [128, 8, TOK], F32, out=True)
    hn2_o = dscr("hn2T", [128, 8, TOK], BF16, out=True)
    aff_o = dscr("affT", [NEXP, TOK], F32, out=True)
    KbT_d = dscr("KbT", [6, 96, S_LEN], BF16)
    Vb_d = dscr("Vb", [6, 128, S_LEN // 128, 128], BF16)
    QbT_d = dscr("QbT", [6, 96, TOK], BF16)
    KaT_d = dscr("KaT", [128, 3, EXT], BF16)
    QaT_d = dscr("QaT", [128, 3, TOK], BF16)
    Va_d = dscr("Va", [6, 128, EXT // 128, 128], BF16)
    U_d = dscr("U", [128, 2, EXT], F32)
    Bc_d = dscr("Bc", [128, 2, TOK], F32)
    mixT_d = dscr("mixT", [D, TOK], F32)

    S = Sched(nc)
    G = Scope(nc)
    ones_bf = G.sb("ones_bf", [128, 128], BF16)
    ones_f = G.sb("ones_f", [128, 128], F32)
    eps_t = G.sb("eps_t", [128, 1], F32)
    bd_bf = G.sb("bd_bf", [128, 128], BF16)
    id_bf = G.sb("id_bf", [128, 128], BF16)
    rot_bf = G.sb("rot_bf", [96, 96], BF16)
    gt = {}
    for name, src, shp in [("g_mix", g_mix_d, [128, 8]), ("g_qa", g_qa_d, [128, 1]), ("g_ka", g_ka_d, [128, 1]),
                           ("g_cq", g_cq_d, [128, 2]), ("g_ckv", g_ckv_d, [128, 1]), ("g_qb", g_qb_d, [96, 1]),
                           ("g_kb", g_kb_d, [96, 1]), ("g_oa", g_oa_d, [64, 6]), ("g_ob", g_ob_d, [64, 6]),
                           ("g_oc", g_oc_d, [128, 2]), ("g_ffn", g_ffn_d, [128, 8]), ("b_rt", b_rt_d, [NEXP, 1])]:
        gt[name] = G.sb(name, shp, F32)
        S.dma("sp", out=gt[name][:], in_=src, writes=[gt[name]], stream="ld")
    convw = G.sb("convw", [128, 2, 3], F32)
    S.dma("sp", out=convw[:], in_=convw_d, writes=[convw], stream="ld")
    S.op("dve", "memset", ones_bf[:], 1.0, writes=[ones_bf])
    S.op("dve", "memset", ones_f[:], 1.0, writes=[ones_f])
    S.op("dve", "memset", eps_t[:], EPS, writes=[eps_t])
    S.dma("pool", out=bd_bf[:], in_=bd_d, writes=[bd_bf], stream="ldc")
    S.dma("pool", out=id_bf[:], in_=ident_d, writes=[id_bf], stream="ldc")
    S.dma("pool", out=rot_bf[:], in_=rot_d, writes=[rot_bf], stream="ldc")
    S.op("dve", "tensor_scalar", out=gt["g_qa"][:], in0=gt["g_qa"][:], scalar1=0.125, scalar2=None, op0=ALU.mult,
         reads=[gt["g_qa"]], writes=[gt["g_qa"]])

    def rstd(ps_ap, out_ap, n, parts, rd, wr):
        S.op("act", "activation", out=out_ap, in_=ps_ap, func=AF.Sqrt, scale=1.0 / n, bias=eps_t[0:parts, :],
             reads=[rd, eps_t], writes=[wr])
        S.op("dve", "reciprocal", out=out_ap, in_=out_ap, reads=[wr], writes=[wr])

    def xprep(P, src_d, t0, xin, xb, xsq, rs_ps, rs_bc):
        S.dma("sp", out=xin[:], in_=src_d[:, :, t0:t0 + 512], writes=[xin], stream="ldx")
        S.op("dve", "tensor_tensor", out=xb[:], in0=xin[:], in1=gt["g_mix"][:].unsqueeze(2).to_broadcast([128, 8, 512]),
             op=ALU.mult, reads=[xin, gt["g_mix"]], writes=[xb])
        S.op("act", "activation", out=xsq[:], in_=xin[:], func=AF.Square, reads=[xin], writes=[xsq])
        for k in range(8):
            S.op("pe", "matmul", rs_ps[:], ones_bf[:], xsq[:, k, :], start=(k == 0), stop=(k == 7),
                 reads=[ones_bf, xsq], writes=[rs_ps])
        rstd(rs_ps[:], rs_bc[:], float(D), 128, rs_ps, rs_bc)

    def proj(ps, wt, c0, ncols, xb, out_parts=None):
        for k in range(8):
            S.op("pe", "matmul", ps[0:ncols, :] if out_parts is None else ps[out_parts, :], wt[:, k, c0:c0 + ncols], xb[:, k, :],
                 start=(k == 0), stop=(k == 7), reads=[wt, xb], writes=[ps])

    def norm_rope(P, raw, g, Ct, St, outt, sq, ss_ps, rs, kbn, rot_ps, t1, t2):
        S.op("act", "activation", out=sq[:], in_=raw[:], func=AF.Square, reads=[raw], writes=[sq])
        S.op("pe", "matmul", ss_ps[0:96, :], ones_bf[0:96, 0:96], sq[:], start=True, stop=True, reads=[ones_bf, sq], writes=[ss_ps])
        rstd(ss_ps[0:96, :], rs[:], 96.0, 96, ss_ps, rs)
        S.op("dve", "scalar_tensor_tensor", out=kbn[:], in0=raw[:], scalar=g[:, 0:1], in1=rs[:], op0=ALU.mult, op1=ALU.mult,
             reads=[raw, g, rs], writes=[kbn])
        S.op("pe", "matmul", rot_ps[0:96, :], rot_bf[:], kbn[:], start=True, stop=True, reads=[rot_bf, kbn], writes=[rot_ps])
        S.op("pool", "tensor_tensor", out=t1[:], in0=kbn[:], in1=Ct[:], op=ALU.mult, reads=[kbn, Ct], writes=[t1])
        S.op("dve", "tensor_tensor", out=t2[:], in0=rot_ps[0:96, :], in1=St[:], op=ALU.mult, reads=[rot_ps, St], writes=[t2])
        S.op("dve", "tensor_tensor", out=outt[:], in0=t1[:], in1=t2[:], op=ALU.add, reads=[t1, t2], writes=[outt])

    def head_norm64(ps, rs_bc, g, outt, tf, sq, ss_ps, rs):
        S.op("dve", "tensor_tensor", out=tf[:], in0=ps[:], in1=rs_bc[:], op=ALU.mult, reads=[ps, rs_bc], writes=[tf])
        S.op("act", "activation", out=sq[:], in_=tf[:], func=AF.Square, reads=[tf], writes=[sq])
        S.op("pe", "matmul", ss_ps[:], bd_bf[:], sq[:], start=True, stop=True, reads=[bd_bf, sq], writes=[ss_ps])
        rstd(ss_ps[:], rs[:], 64.0, 128, ss_ps, rs)
        S.op("dve", "scalar_tensor_tensor", out=outt[:], in0=tf[:], scalar=g[:, 0:1], in1=rs[:], op0=ALU.mult, op1=ALU.mult,
             reads=[tf, g, rs], writes=[outt])

    P = Scope(nc)
    wA = P.sb("wA_ckv", [128, 8, 128], BF16)
    wKr = P.sb("wA_kr", [128, 8, 96], BF16)
    wk = P.sb("wukv_k", [128, 6, 64], BF16)
    wv = P.sb("wukv_v", [128, 6, 64], BF16)
    S.dma("pool", out=wA[:], in_=w_in_d[:, C_CKV:C_CKV + 128].rearrange("(c p) f -> p c f", p=128), writes=[wA], stream="ldc")
    S.op("dve", "memset", wKr[:], 0.0, writes=[wKr])
    S.dma("pool", out=wKr[:, :, 64:96], in_=w_in_d[:, C_KR:C_KR + 32].rearrange("(c p) f -> p c f", p=128), writes=[wKr], stream="ldc")
    ukv3 = w_ukv_d.rearrange("r (h x) -> r h x", x=128)
    S.dma("pool", out=wk[:], in_=ukv3[:, :, 0:64], writes=[wk], stream="ldc")
    S.dma("pool", out=wv[:], in_=ukv3[:, :, 64:128], writes=[wv], stream="ldc")
    xin = [P.sb(f"xin{i}", [128, 8, 512], F32) for i in range(2)]
    xb = [P.sb(f"xb{i}", [128, 8, 512], BF16) for i in range(2)]
    xsq = [P.sb(f"xsq{i}", [128, 8, 512], BF16) for i in range(2)]
    rs_bc = [P.sb(f"rsbc{i}", [128, 512], F32) for i in range(2)]
    ckv = P.sb("ckv", [128, 512], F32)
    sqc = P.sb("sqc", [128, 512], BF16)
    rsc = P.sb("rsc", [128, 512], F32)
    cn = [P.sb(f"cn{i}", [128, 512], BF16) for i in range(2)]
    raw = [P.sb(f"raw{i}", [96, 512], F32) for i in range(2)]
    Ct = [P.sb(f"Ct{i}", [96, 512], F32) for i in range(2)]
    St = [P.sb(f"St{i}", [96, 512], F32) for i in range(2)]
    sq96 = P.sb("sq96", [96, 512], BF16)
    rs96 = P.sb("rs96", [96, 512], F32)
    kbn = P.sb("kbn", [96, 512], BF16)
    t1 = P.sb("t1", [96, 512], F32)
    t2 = P.sb("t2", [96, 512], F32)
    ko = [P.sb(f"ko{i}", [96, 512], BF16) for i in range(2)]
    Vst = [P.sb(f"Vst{i}", [128, 6, 4, 128], BF16) for i in range(2)]
    rs_ps = P.ps("rs_ps")
    pj_ps = [P.ps(f"pj_ps{i}") for i in range(2)]
    ss_ps = P.ps("ss_ps")
    rot_ps = P.ps("rot_ps")
    v_ps = [P.ps(f"v_ps{i}") for i in range(2)]
    for i in range(2):
        S.op("dve", "memset", Vst[i][:], 1.0, writes=[Vst[i]])
    for blk in range(S_LEN // 512):
        i = blk % 2
        t0 = blk * 512
        xprep(P, xTf_d, t0, xin[i], xb[i], xsq[i], rs_ps, rs_bc[i])
        S.dma("sp", out=Ct[i][:], in_=ropeKC_d[:, t0:t0 + 512], writes=[Ct[i]], stream="ldx")
        S.dma("sp", out=St[i][:], in_=ropeKS_d[:, t0:t0 + 512], writes=[St[i]], stream="ldx")
        proj(pj_ps[0], wA, 0, 128, xb[i])
        S.op("dve", "tensor_tensor", out=ckv[:], in0=pj_ps[0][:], in1=rs_bc[i][:], op=ALU.mult, reads=[pj_ps[0], rs_bc[i]], writes=[ckv])
        proj(pj_ps[1], wKr, 0, 96, xb[i])
        for r in range(2):
            S.op("dve", "tensor_tensor", out=raw[r][64:96, :], in0=pj_ps[1][64:96, :], in1=rs_bc[i][64:96, :], op=ALU.mult,
                 reads=[pj_ps[1], rs_bc[i]], writes=[raw[r]])
        S.op("act", "activation", out=sqc[:], in_=ckv[:], func=AF.Square, reads=[ckv], writes=[sqc])
        S.op("pe", "matmul", ss_ps[:], ones_bf[:], sqc[:], start=True, stop=True, reads=[ones_bf, sqc], writes=[ss_ps])
        rstd(ss_ps[:], rsc[:], 128.0, 128, ss_ps, rsc)
        S.op("dve", "scalar_tensor_tensor", out=cn[i][:], in0=ckv[:], scalar=gt["g_ckv"][:, 0:1], in1=rsc[:], op0=ALU.mult, op1=ALU.mult,
             reads=[ckv, gt["g_ckv"], rsc], writes=[cn[i]])
        for sub in range(4):
            vp = v_ps[sub % 2]
            S.op("pe", "matmul", vp[:, 0:384], cn[i][:, sub * 128:(sub + 1) * 128], wv[:].rearrange("p h x -> p (h x)"),
                 start=True, stop=True, reads=[cn[i], wv], writes=[vp])
            S.op("act", "activation", out=Vst[i][:, :, sub, 0:64], in_=vp[:, 0:384].rearrange("p (h x) -> p h x", x=64), func=AF.Copy,
                 reads=[vp], writes=[Vst[i]])
        S.dma("pool", out=Vb_d[:, :, blk * 4:(blk + 1) * 4, :].rearrange("h p c x -> p h c x"), in_=Vst[i][:], reads=[Vst[i]], stream="st")
        for h in range(6):
            r = h % 2
            S.op("pe", "matmul", pj_ps[r][0:64, :], wk[:, h, :], cn[i][:], start=True, stop=True, reads=[wk, cn[i]], writes=[pj_ps[r]])
            S.op("act", "activation", out=raw[r][0:64, :], in_=pj_ps[r][0:64, :], func=AF.Copy, reads=[pj_ps[r]], writes=[raw[r]])
            norm_rope(P, raw[r], gt["g_kb"], Ct[i], St[i], ko[r], sq96, ss_ps, rs96, kbn, rot_ps, t1, t2)
            S.dma("pool", out=KbT_d[h, :, t0:t0 + 512], in_=ko[r][:], reads=[ko[r]], stream="st")
    S.barrier()
    P.close()

    P = Scope(nc)
    wB = P.sb("wB", [128, 8, 2336], BF16)
    S.dma("pool", out=wB[:], in_=w_in_d.rearrange("(c p) f -> p c f", p=128), writes=[wB], stream="ldc")
    wuq = P.sb("wuq", [128, 2, 576], BF16)
    S.dma("pool", out=wuq[:], in_=w_uq_d.rearrange("(c p) f -> p c f", p=128), writes=[wuq], stream="ldc")
    xin = [P.sb(f"xin{i}", [128, 8, 512], F32) for i in range(2)]
    xb = [P.sb(f"xb{i}", [128, 8, 512], BF16) for i in range(2)]
    xsq = [P.sb(f"xsq{i}", [128, 8, 512], BF16) for i in range(2)]
    rs_bc = [P.sb(f"rsbc{i}", [128, 512], F32) for i in range(2)]
    tf = P.sb("tf", [128, 512], F32)
    sq = P.sb("sq", [128, 512], BF16)
    rs = P.sb("rs", [128, 512], F32)
    hout = [P.sb(f"hout{i}", [128, 512], BF16) for i in range(2)]
    rtm = P.sb("rtm", [128, 4], F32)
    Vst = [P.sb(f"Vst{i}", [128, 6, 4, 128], BF16) for i in range(2)]
    th = P.sb("th", [128, 512], F32)
    tc_ = P.sb("tc", [128, 512], F32)
    uo = [P.sb(f"uo{i}", [128, 512], F32) for i in range(2)]
    cq = P.sb("cq", [128, 2, 512], F32)
    sq2 = P.sb("sq2", [128, 2, 512], BF16)
    cqn = P.sb("cqn", [128, 2, 512], BF16)
    raw = [P.sb(f"raw{i}", [96, 512], F32) for i in range(2)]
    Ct = P.sb("Ct", [96, 512], F32)
    St = P.sb("St", [96, 512], F32)
    sq96 = P.sb("sq96", [96, 512], BF16)
    rs96 = P.sb("rs96", [96, 512], F32)
    kbn = P.sb("kbn", [96, 512], BF16)
    t1 = P.sb("t1", [96, 512], F32)
    t2 = P.sb("t2", [96, 512], F32)
    ko = [P.sb(f"ko{i}", [96, 512], BF16) for i in range(2)]
    rs_ps = P.ps("rs_ps")
    pj_ps = [P.ps(f"pj_ps{i}") for i in range(2)]
    ss_ps = P.ps("ss_ps")
    rot_ps = P.ps("rot_ps")
    v_ps = [P.ps(f"v_ps{i}") for i in range(2)]
    tm_ps = P.ps("tm_ps")
    for i in range(2):
        S.op("dve", "memset", Vst[i][:], 1.0, writes=[Vst[i]])
    npj = [0]

    def nextps():
        npj[0] += 1
        return pj_ps[npj[0] % 2]
    nh = [0]

    def nexth():
        nh[0] += 1
        return hout[nh[0] % 2]
    for blk in range(EXT // 512):
        i = blk % 2
        t0 = blk * 512
        own = 1 <= blk <= NQB
        q0 = t0 - 512
        xprep(P, xTe_d, t0, xin[i], xb[i], xsq[i], rs_ps, rs_bc[i])
        for p in range(3):
            ps = nextps()
            proj(ps, wB, C_KA + p * 128, 128, xb[i])
            ho = nexth()
            head_norm64(ps, rs_bc[i], gt["g_ka"], ho, tf, sq, ss_ps, rs)
            S.dma("pool", out=KaT_d[:, p, t0:t0 + 512], in_=ho[:], reads=[ho], stream="st")
        for sub in range(4):
            for k in range(8):
                S.op("pe", "matmul", tm_ps[:, sub:sub + 1], xsq[i][:, k, sub * 128:(sub + 1) * 128], ones_bf[:, 0:1],
                     start=(k == 0), stop=(k == 7), reads=[xsq[i], ones_bf], writes=[tm_ps])
        rstd(tm_ps[:, 0:4], rtm[:], float(D), 128, tm_ps, rtm)
        for sub in range(4):
            vp = v_ps[sub % 2]
            for k in range(8):
                S.op("pe", "matmul", vp[:, 0:384], xb[i][:, k, sub * 128:(sub + 1) * 128], wB[:, k, C_VA:C_VA + 384],
                     start=(k == 0), stop=(k == 7), reads=[xb[i], wB], writes=[vp])
            S.op("act", "activation", out=Vst[i][:, :, sub, 0:64], in_=vp[:, 0:384].rearrange("p (h x) -> p h x", x=64), func=AF.Copy,
                 scale=rtm[:, sub:sub + 1], reads=[vp, rtm], writes=[Vst[i]])
        S.dma("pool", out=Va_d[:, :, blk * 4:(blk + 1) * 4, :].rearrange("h p c x -> p h c x"), in_=Vst[i][:], reads=[Vst[i]], stream="st")
        for c in range(2):
            ph = nextps()
            proj(ph, wB, C_HC + c * 128, 128, xb[i])
            S.op("dve", "tensor_tensor", out=th[:], in0=ph[:], in1=rs_bc[i][:], op=ALU.mult, reads=[ph, rs_bc[i]], writes=[th])
            pc = nextps()
            proj(pc, wB, C_CC + c * 128, 128, xb[i])
            S.op("dve", "tensor_tensor", out=tc_[:], in0=pc[:], in1=rs_bc[i][:], op=ALU.mult, reads=[pc, rs_bc[i]], writes=[tc_])
            u_ = uo[c]
            S.op("pool", "tensor_tensor", out=u_[:], in0=th[:], in1=tc_[:], op=ALU.mult, reads=[th, tc_], writes=[u_])
            S.dma("pool", out=U_d[:, c, t0:t0 + 512], in_=u_[:], reads=[u_], stream="st")
        if not own:
            continue
        for p in range(3):
            ps = nextps()
            proj(ps, wB, C_QA + p * 128, 128, xb[i])
            ho = nexth()
            head_norm64(ps, rs_bc[i], gt["g_qa"], ho, tf, sq, ss_ps, rs)
            S.dma("pool", out=QaT_d[:, p, q0:q0 + 512], in_=ho[:], reads=[ho], stream="st")
        for c in range(2):
            ps = nextps()
            proj(ps, wB, C_BC + c * 128, 128, xb[i])
            u_ = uo[c]
            S.op("dve", "tensor_tensor", out=u_[:], in0=ps[:], in1=rs_bc[i][:], op=ALU.mult, reads=[ps, rs_bc[i]], writes=[u_])
            S.dma("pool", out=Bc_d[:, c, q0:q0 + 512], in_=u_[:], reads=[u_], stream="st")
        for c in range(2):
            ps = nextps()
            proj(ps, wB, C_CQ + c * 128, 128, xb[i])
            S.op("dve", "tensor_tensor", out=cq[:, c, :], in0=ps[:], in1=rs_bc[i][:], op=ALU.mult, reads=[ps, rs_bc[i]], writes=[cq])
        S.op("act", "activation", out=sq2[:], in_=cq[:], func=AF.Square, reads=[cq], writes=[sq2])
        for c in range(2):
            S.op("pe", "matmul", ss_ps[:], ones_bf[:], sq2[:, c, :], start=(c == 0), stop=(c == 1), reads=[ones_bf, sq2], writes=[ss_ps])
        rstd(ss_ps[:], rs[:], 256.0, 128, ss_ps, rs)
        for c in range(2):
            S.op("dve", "scalar_tensor_tensor", out=cqn[:, c, :], in0=cq[:, c, :], scalar=gt["g_cq"][:, c:c + 1], in1=rs[:],
                 op0=ALU.mult, op1=ALU.mult, reads=[cq, gt["g_cq"], rs], writes=[cqn])
        S.dma("sp", out=Ct[:], in_=ropeQC_d[:, q0:q0 + 512], writes=[Ct], stream="ldx")
        S.dma("sp", out=St[:], in_=ropeQS_d[:, q0:q0 + 512], writes=[St], stream="ldx")
        for h in range(6):
            r = h % 2
            ps = nextps()
            for c in range(2):
                S.op("pe", "matmul", ps[0:96, :], wuq[:, c, h * 96:(h + 1) * 96], cqn[:, c, :], start=(c == 0), stop=(c == 1),
                     reads=[wuq, cqn], writes=[ps])
            S.op("act", "activation", out=raw[r][:], in_=ps[0:96, :], func=AF.Copy, reads=[ps], writes=[raw[r]])
            norm_rope(P, raw[r], gt["g_qb"], Ct, St, ko[r], sq96, ss_ps, rs96, kbn, rot_ps, t1, t2)
            S.dma("pool", out=QbT_d[h, :, q0:q0 + 512], in_=ko[r][:], reads=[ko[r]], stream="st")
    S.barrier()
    P.close()

    def finalize(po, rec, on, dst_ap):
        S.op("dve", "reciprocal", out=rec[64:128, :], in_=po[64:128, :], reads=[po], writes=[rec])
        S.op("dve", "tensor_tensor", out=on[:], in0=po[0:64, :], in1=rec[64:128, :], op=ALU.mult, reads=[po, rec], writes=[on])
        S.dma("pool", out=dst_ap, in_=on[:], reads=[on], stream="st")

    P = Scope(nc)
    Kt = [P.sb(f"Kt{i}", [128, 1024], BF16) for i in range(2)]
    Qt = [P.sb(f"Qt{i}", [128, 512], BF16) for i in range(2)]
    Vt = [P.sb(f"Vt{i}", [128, 8, 128], BF16) for i in range(2)]
    Bt = [P.sb(f"Bt{i}", [128, 8, 512], BF16) for i in range(2)]
    Pt = [P.sb(f"Pt{i}", [128, 512], BF16) for i in range(3)]
    rec = P.sb("rec", [128, 512], F32)
    on = [P.sb(f"on{i}", [64, 512], F32) for i in range(2)]
    s_ps = [P.ps(f"s_ps{i}") for i in range(3)]
    o_ps = [P.ps(f"o_ps{i}") for i in range(2)]
    n = 0
    it = 0
    for j in range(NQB):
        var = 0 if j == 0 else (2 if j == NQB - 1 else 1)
        k0 = 512 * j + 256
        for p in range(3):
            kt, qt = Kt[(j * 3 + p) % 2], Qt[(j * 3 + p) % 2]
            S.dma("sp", out=kt[:], in_=KaT_d[:, p, k0:k0 + 1024], writes=[kt], stream="ldx")
            S.dma("sp", out=qt[:], in_=QaT_d[:, p, j * 512:(j + 1) * 512], writes=[qt], stream="ldx")
            for hh in range(2):
                h = p * 2 + hh
                vt, bt, po = Vt[it % 2], Bt[it % 2], o_ps[it % 2]
                it += 1
                S.dma("sp", out=vt[:], in_=Va_d[h, :, k0 // 128:k0 // 128 + 8, :], writes=[vt], stream="ldx")
                S.dma("pool", out=bt[:], in_=biasA_d[var, h], writes=[bt], stream="ldb")
                pp = slice(hh * 64, (hh + 1) * 64)
                for c in range(8):
                    sp_, pt = s_ps[n % 3], Pt[n % 3]
                    n += 1
                    S.op("pe", "matmul", sp_[:], kt[pp, c * 128:(c + 1) * 128], qt[pp, :], start=True, stop=False, reads=[kt, qt], writes=[sp_])
                    S.op("pe", "matmul", sp_[:], id_bf[:], bt[:, c, :], start=False, stop=True, reads=[id_bf, bt], writes=[sp_])
                    S.op("act", "activation", out=pt[:], in_=sp_[:], func=AF.Exp, reads=[sp_], writes=[pt])
                    S.op("pe", "matmul", po[:], vt[:, c, :], pt[:], start=(c == 0), stop=(c == 7), reads=[vt, pt], writes=[po])
                finalize(po, rec, on[it % 2], mixT_d[h * 64:(h + 1) * 64, j * 512:(j + 1) * 512])
    S.barrier()
    P.close()

    P = Scope(nc)
    Kh = [P.sb(f"Kh{i}", [96, S_LEN], BF16) for i in range(2)]
    Vh = [P.sb(f"Vh{i}", [128, S_LEN // 128, 128], BF16) for i in range(2)]
    Qh = [P.sb(f"Qh{i}", [96, TOK], BF16) for i in range(2)]
    Pt = [P.sb(f"Pt{i}", [128, 512], BF16) for i in range(4)]
    rec = P.sb("rec", [128, 512], F32)
    on = [P.sb(f"on{i}", [64, 512], F32) for i in range(2)]
    s_ps = [P.ps(f"s_ps{i}") for i in range(4)]
    o_ps = [P.ps(f"o_ps{i}") for i in range(2)]
    n = 0
    it = 0
    sc_b = 96.0 ** -0.5
    for h in range(6):
        kh, vh, qh = Kh[h % 2], Vh[h % 2], Qh[h % 2]
        S.dma("sp", out=kh[:], in_=KbT_d[h], writes=[kh], stream="ldx")
        S.dma("sp", out=vh[:], in_=Vb_d[h], writes=[vh], stream="ldx")
        S.dma("sp", out=qh[:], in_=QbT_d[h], writes=[qh], stream="ldx")
        for j in range(NQB):
            po = o_ps[it % 2]
            it += 1
            for c in range(S_LEN // 128):
                sp_, pt = s_ps[n % 4], Pt[n % 4]
                n += 1
                S.op("pe", "matmul", sp_[:], kh[:, c * 128:(c + 1) * 128], qh[:, j * 512:(j + 1) * 512], start=True, stop=True,
                     reads=[kh, qh], writes=[sp_])
                S.op("act", "activation", out=pt[:], in_=sp_[:], func=AF.Exp, scale=sc_b, reads=[sp_], writes=[pt])
                S.op("pe", "matmul", po[:], vh[:, c, :], pt[:], start=(c == 0), stop=(c == S_LEN // 128 - 1), reads=[vh, pt], writes=[po])
            finalize(po, rec, on[it % 2], mixT_d[384 + h * 64:384 + (h + 1) * 64, j * 512:(j + 1) * 512])
    S.barrier()
    P.close()

    P = Scope(nc)
    woA = P.sb("woA", [64, 6, D], BF16)
    woB = P.sb("woB", [64, 6, D], BF16)
    woC = P.sb("woC", [128, 2, D], BF16)
    wrt = P.sb("wrt", [128, 8, NEXP], F32)
    S.dma("pool", out=woA[:], in_=w_out_d[0:384, :].rearrange("(h p) d -> p h d", p=64), writes=[woA], stream="ldc")
    S.dma("pool", out=woB[:], in_=w_out_d[384:768, :].rearrange("(h p) d -> p h d", p=64), writes=[woB], stream="ldc")
    S.dma("pool", out=woC[:], in_=w_out_d[768:1024, :].rearrange("(c p) d -> p c d", p=128), writes=[woC], stream="ldc")
    S.dma("sp", out=wrt[:], in_=w_rt_d.rearrange("(c p) e -> p c e", p=128), writes=[wrt], stream="ld")
    ut = [P.sb(f"ut{i}", [128, 2, 514], F32) for i in range(1)]
    bct = [P.sb(f"bct{i}", [128, 2, 512], F32) for i in range(1)]
    yc = P.sb("yc", [128, 2, 512], F32)
    mA = [P.sb(f"mA{i}", [64, 6, 512], F32) for i in range(1)]
    mB = [P.sb(f"mB{i}", [64, 6, 512], F32) for i in range(1)]
    xo = [P.sb(f"xo{i}", [128, 8, 512], F32) for i in range(1)]
    sqA = P.sb("sqA", [64, 6, 512], BF16)
    sqC = P.sb("sqC", [128, 2, 512], BF16)
    rsn = P.sb("rsn", [128, 512], F32)
    tA = P.sb("tA", [64, 6, 512], F32)
    nA = P.sb("nA", [64, 6, 512], BF16)
    nB = P.sb("nB", [64, 6, 512], BF16)
    nC = P.sb("nC", [128, 2, 512], BF16)
    xm = [P.sb(f"xm{i}", [128, 8, 512], F32) for i in range(1)]
    sqx = P.sb("sqx", [128, 8, 512], BF16)
    hf = P.sb("hf", [128, 8, 512], F32)
    hb_ = [P.sb(f"hb{i}", [128, 8, 512], BF16) for i in range(1)]
    ex = P.sb("ex", [NEXP, 512], F32)
    rsum = P.sb("rsum", [NEXP, 512], F32)
    affo = [P.sb(f"affo{i}", [NEXP, 512], F32) for i in range(1)]
    ss_ps = P.ps("ss_ps")
    w_ps = [P.ps(f"w_ps{i}") for i in range(2)]
    r_ps = P.ps("r_ps")
    mAd = mixT_d[0:384, :].rearrange("(h p) t -> p h t", p=64)
    mBd = mixT_d[384:768, :].rearrange("(h p) t -> p h t", p=64)
    mCd = mixT_d[768:1024, :].rearrange("(c p) t -> p c t", p=128)

    def seg_norm(src, nchunk, parts, g, sqt, tmp, outt, n):
        S.op("act", "activation", out=sqt[:], in_=src[:], func=AF.Square, reads=[src], writes=[sqt])
        for c in range(nchunk):
            S.op("pe", "matmul", ss_ps[0:parts, :], ones_bf[0:parts, 0:parts], sqt[:, c, :], start=(c == 0), stop=(c == nchunk - 1),
                 reads=[ones_bf, sqt], writes=[ss_ps])
        rstd(ss_ps[0:parts, :], rsn[0:parts, :], float(n), parts, ss_ps, rsn)
        S.op("dve", "tensor_tensor", out=tmp[:], in0=src[:], in1=g[:].unsqueeze(2).to_broadcast([parts, nchunk, 512]), op=ALU.mult,
             reads=[src, g], writes=[tmp])
        S.op("dve", "tensor_tensor", out=outt[:], in0=tmp[:], in1=rsn[0:parts, :].unsqueeze(1).to_broadcast([parts, nchunk, 512]), op=ALU.mult,
             reads=[tmp, rsn], writes=[outt])

    for j in range(NQB):
        i = 0
        e0 = 512 * (j + 1)
        S.dma("sp", out=ut[i][:], in_=U_d[:, :, e0 - 1:e0 + 513], writes=[ut[i]], stream="ldx")
        S.dma("sp", out=bct[i][:], in_=Bc_d[:, :, j * 512:(j + 1) * 512], writes=[bct[i]], stream="ldx")
        for c in range(2):
            S.op("dve", "tensor_scalar", out=yc[:, c, :], in0=ut[i][:, c, 0:512], scalar1=convw[:, c, 0:1], scalar2=None, op0=ALU.mult,
                 reads=[ut[i], convw], writes=[yc])
            for kk in (1, 2):
                S.op("dve", "scalar_tensor_tensor", out=yc[:, c, :], in0=ut[i][:, c, kk:kk + 512], scalar=convw[:, c, kk:kk + 1], in1=yc[:, c, :],
                     op0=ALU.mult, op1=ALU.add, reads=[ut[i], convw, yc], writes=[yc])
        S.op("dve", "tensor_tensor", out=yc[:], in0=yc[:], in1=bct[i][:], op=ALU.mult, reads=[yc, bct[i]], writes=[yc])
        S.dma("sp", out=mA[i][:], in_=mAd[:, :, j * 512:(j + 1) * 512], writes=[mA[i]], stream="ldx")
        S.dma("sp", out=mB[i][:], in_=mBd[:, :, j * 512:(j + 1) * 512], writes=[mB[i]], stream="ldx")
        S.dma("sp", out=xo[i][:], in_=xTe_d[:, :, e0:e0 + 512], writes=[xo[i]], stream="ldx")
        seg_norm(mA[i], 6, 64, gt["g_oa"], sqA, tA, nA, 384)
        seg_norm(mB[i], 6, 64, gt["g_ob"], sqA, tA, nB, 384)
        seg_norm(yc, 2, 128, gt["g_oc"], sqC, tA, nC, 256) if False else None
        S.op("act", "activation", out=sqC[:], in_=yc[:], func=AF.Square, reads=[yc], writes=[sqC])
        for c in range(2):
            S.op("pe", "matmul", ss_ps[:], ones_bf[:], sqC[:, c, :], start=(c == 0), stop=(c == 1), reads=[ones_bf, sqC], writes=[ss_ps])
        rstd(ss_ps[:], rsn[:], 256.0, 128, ss_ps, rsn)
        for c in range(2):
            S.op("dve", "scalar_tensor_tensor", out=nC[:, c, :], in0=yc[:, c, :], scalar=gt["g_oc"][:, c:c + 1], in1=rsn[:],
                 op0=ALU.mult, op1=ALU.mult, reads=[yc, gt["g_oc"], rsn], writes=[nC])
        for dc in range(8):
            wp = w_ps[dc % 2]
            dsl = slice(dc * 128, (dc + 1) * 128)
            for h in range(6):
                S.op("pe", "matmul", wp[:], woA[:, h, dsl], nA[:, h, :], start=(h == 0), stop=False, reads=[woA, nA], writes=[wp])
            for h in range(6):
                S.op("pe", "matmul", wp[:], woB[:, h, dsl], nB[:, h, :], start=False, stop=False, reads=[woB, nB], writes=[wp])
            for c in range(2):
                S.op("pe", "matmul", wp[:], woC[:, c, dsl], nC[:, c, :], start=False, stop=(c == 1), reads=[woC, nC], writes=[wp])
            S.op("dve", "tensor_tensor", out=xm[i][:, dc, :], in0=wp[:], in1=xo[i][:, dc, :], op=ALU.add, reads=[wp, xo[i]], writes=[xm[i]])
        S.dma("pool", out=xmid_o[:, :, j * 512:(j + 1) * 512], in_=xm[i][:], reads=[xm[i]], stream="st")
        S.op("act", "activation", out=sqx[:], in_=xm[i][:], func=AF.Square, reads=[xm[i]], writes=[sqx])
        for k in range(8):
            S.op("pe", "matmul", ss_ps[:], ones_bf[:], sqx[:, k, :], start=(k == 0), stop=(k == 7), reads=[ones_bf, sqx], writes=[ss_ps])
        rstd(ss_ps[:], rsn[:], float(D), 128, ss_ps, rsn)
        S.op("dve", "tensor_tensor", out=hf[:], in0=xm[i][:], in1=gt["g_ffn"][:].unsqueeze(2).to_broadcast([128, 8, 512]), op=ALU.mult,
             reads=[xm[i], gt["g_ffn"]], writes=[hf])
        S.op("dve", "tensor_tensor", out=hf[:], in0=hf[:], in1=rsn[:].unsqueeze(1).to_broadcast([128, 8, 512]), op=ALU.mult,
             reads=[hf, rsn], writes=[hf])
        S.op("act", "activation", out=hb_[i][:], in_=hf[:], func=AF.Copy, reads=[hf], writes=[hb_[i]])
        S.dma("pool", out=hn2_o[:, :, j * 512:(j + 1) * 512], in_=hb_[i][:], reads=[hb_[i]], stream="st")
        for k in range(8):
            S.op("pe", "matmul", r_ps[0:NEXP, :], wrt[:, k, :], hf[:, k, :], start=(k == 0), stop=(k == 7), reads=[wrt, hf], writes=[r_ps])
        S.op("act", "activation", out=ex[:], in_=r_ps[0:NEXP, :], func=AF.Exp, bias=gt["b_rt"][:, 0:1], reads=[r_ps, gt["b_rt"]], writes=[ex])
        S.op("pe", "matmul", r_ps[0:NEXP, :], ones_f[0:NEXP, 0:NEXP], ex[:], start=True, stop=True, reads=[ones_f, ex], writes=[r_ps])
        S.op("dve", "reciprocal", out=rsum[:], in_=r_ps[0:NEXP, :], reads=[r_ps], writes=[rsum])
        S.op("dve", "tensor_tensor", out=affo[i][:], in0=ex[:], in1=rsum[:], op=ALU.mult, reads=[ex, rsum], writes=[affo[i]])
        S.dma("pool", out=aff_o[:, j * 512:(j + 1) * 512], in_=affo[i][:], reads=[affo[i]], stream="st")
    S.finish()
    P.close()
    G.close()
    return nc


def _chunkT(a):
    t, f = a.shape
    return np.ascontiguousarray(a.T.reshape(f // 128, 128, t).transpose(1, 0, 2))


def _rope_tables():
    half = 16
    inv = (np.float32(10000.0) ** (-(np.arange(half, dtype=np.float32)) / np.float32(half))).astype(np.float32)
    ang = np.arange(S_LEN, dtype=np.float32)[:, None] * inv[None, :]
    cos = np.cos(ang).astype(np.float32).T
    sin = np.sin(ang).astype(np.float32).T
    C = np.ones((96, S_LEN), np.float32)
    Sn = np.zeros((96, S_LEN), np.float32)
    C[64:80] = cos
    C[80:96] = cos
    Sn[64:80] = sin
    Sn[80:96] = sin
    rotT = np.zeros((96, 96), np.float32)
    for i in range(16):
        rotT[80 + i, 64 + i] = -1.0
        rotT[64 + i, 80 + i] = 1.0
    return C, Sn, rotT


def _bias_block(rpb, rb):
    qr = np.arange(8)
    r = rb + qr
    r0 = np.clip(r - 4, 0, 256 - 8)
    kr = rb - 4 + np.arange(16)
    valid_r = (kr[None, :] >= r0[:, None]) & (kr[None, :] < r0[:, None] + 8)
    row_off = np.clip(kr[None, :] - r[:, None] + 7, 0, 14)
    qc = np.arange(64)
    c0 = np.clip(qc - 8, 0, 64 - 16)
    kc = np.arange(64)
    valid_c = (kc[None, :] >= c0[:, None]) & (kc[None, :] < c0[:, None] + 16)
    col_off = np.clip(kc[None, :] - qc[:, None] + 15, 0, 30)
    g = rpb[:, row_off.T[:, None, :, None], col_off.T[None, :, None, :]]
    valid = valid_r.T[:, None, :, None] & valid_c.T[None, :, None, :]
    out = np.where(valid[None], g, np.float32(-30000.0)).astype(np.float32)
    out = out.reshape(6, 8, 128, 512).transpose(0, 2, 1, 3)
    return np.ascontiguousarray(out)


_PROGS = {}


def _prog(name):
    if name not in _PROGS:
        _PROGS[name] = build_mix() if name == "mix" else build_moe()
    return _PROGS[name]


def _mix_maps(x, l, p, consts):
    C, Sn, rotT, bd, ident = consts
    maps = []
    g1 = lambda v: np.ascontiguousarray(v.reshape(-1, 128).T)
    common = {
        "w_in": p["w_in"][l], "w_uq": p["w_uq"][l], "w_ukv": p["w_ukv"][l], "w_out": p["w_out"][l],
        "w_router": p["w_router"][l],
        "g_mix": g1(p["norm_mix"][l]),
        "g_qa": np.ascontiguousarray(np.tile(p["q_norm_a"][l], 2)[:, None]),
        "g_ka": np.ascontiguousarray(np.tile(p["k_norm_a"][l], 2)[:, None]),
        "g_cq": g1(p["cq_norm"][l]), "g_ckv": g1(p["ckv_norm"][l]),
        "g_qb": np.ascontiguousarray(p["q_norm_b"][l][:, None]), "g_kb": np.ascontiguousarray(p["k_norm_b"][l][:, None]),
        "conv_w": np.ascontiguousarray(p["conv_w"][l].T.reshape(2, 128, 3).transpose(1, 0, 2)),
        "g_oa": np.ascontiguousarray(p["out_norm"][l][0:384].reshape(6, 64).T),
        "g_ob": np.ascontiguousarray(p["out_norm"][l][384:768].reshape(6, 64).T),
        "g_oc": g1(p["out_norm"][l][768:1024]),
        "g_ffn": g1(p["norm_ffn"][l]), "b_router": np.ascontiguousarray(p["b_router"][l][:, None]),
        "ropeKC": C, "ropeKS": Sn, "rotT": rotT, "blockdiag": bd, "ident": ident,
    }
    rpb = p["rpb"][l]
    b_int = _bias_block(rpb, 64)
    xTf = [_chunkT(x[b]) for b in range(2)]
    for core in range(NCORES):
        b, q = divmod(core, 4)
        xpad = np.zeros((S_LEN + 1024, D), np.float32)
        xpad[512:512 + S_LEN] = x[b]
        m = dict(common)
        m["xT_full"] = xTf[b]
        m["xT_ext"] = _chunkT(xpad[q * TOK:q * TOK + EXT])
        m["ropeQC"] = np.ascontiguousarray(C[:, q * TOK:(q + 1) * TOK])
        m["ropeQS"] = np.ascontiguousarray(Sn[:, q * TOK:(q + 1) * TOK])
        b0 = _bias_block(rpb, 0) if q == 0 else b_int
        b2 = _bias_block(rpb, 248) if q == 3 else b_int
        m["biasA"] = np.ascontiguousarray(np.stack([b0, b_int, b2]))
        maps.append(m)
    return maps


def _run_layer(x, l, p, consts):
    res = run_bass_kernel_spmd(_prog("mix"), _mix_maps(x, l, p, consts), core_ids=list(range(NCORES))).results
    maps = []
    for core in range(NCORES):
        b, q = divmod(core, 4)
        affb = np.concatenate([res[b * 4 + i]["affT"] for i in range(4)], axis=1)
        aff_own = res[core]["affT"]
        xm = res[core]["xmidT"]
        xm_tok = xm.transpose(2, 1, 0).reshape(TOK, D)
        maps.append({
            "aff_full": np.ascontiguousarray(affb.T.reshape(128, 128 * NEXP)),
            "aff_own": np.ascontiguousarray(aff_own.T.reshape(32, 128, NEXP).transpose(1, 0, 2).reshape(128, 32 * NEXP)),
            "hn2T": res[core]["hn2T"],
            "xmid": np.ascontiguousarray(xm_tok.reshape(32, 128, D).transpose(1, 0, 2)),
            "wg": p["w_gate"][l], "wu": p["w_up"][l], "wd": p["w_down"][l],
        })
    res2 = run_bass_kernel_spmd(_prog("moe"), maps, core_ids=list(range(NCORES))).results
    xn = np.empty((2, S_LEN, D), np.float32)
    for core in range(NCORES):
        b, q = divmod(core, 4)
        xn[b, q * TOK:(q + 1) * TOK] = res2[core]["xout"].transpose(1, 0, 2).reshape(TOK, D)
    return xn, res


def kernel(**inputs):
    p = {k: np.asarray(v, dtype=np.float32) for k, v in inputs.items()}
    x = p.pop("x")
    C, Sn, rotT = _rope_tables()
    bd = np.zeros((128, 128), np.float32)
    bd[:64, :64] = 1.0
    bd[64:, 64:] = 1.0
    consts = (C, Sn, rotT, bd, np.eye(128, dtype=np.float32))
    for l in range(2):
        x, _ = _run_layer(x, l, p, consts)
    return x
```

```python
import contextlib
import numpy as np
import ml_dtypes
import concourse.bass as bass
import concourse.mybir as mybir
from concourse.bass_utils import run_bass_kernel_spmd

F32 = mybir.dt.float32
BF16 = mybir.dt.bfloat16
AF = mybir.ActivationFunctionType
ALU = mybir.AluOpType
AX = mybir.AxisListType

NCORES = 8
D = 1024
S_LEN = 16384
TOK = 4096
NEXP = 16
CAP = 2048
EPS = 1e-6


class Buf:
    __slots__ = ("name", "w", "r")

    def __init__(self, name=""):
        self.name = name
        self.w = None
        self.r = {}


class Tl:
    def __init__(self, nc, name, shape, dtype, psum=False):
        if psum:
            self.t = nc.alloc_psum_tensor(name, shape, dtype)
        else:
            self.t = nc.alloc_sbuf_tensor(name, shape, dtype)
        self.b = Buf(name)

    def __getitem__(self, idx):
        return self.t[idx]


def _b(x):
    return x.b if hasattr(x, "b") else x


class Sched:
    ENGS = ("pe", "act", "dve", "pool", "sp")

    def __init__(self, nc):
        self.nc = nc
        self.ops = {e: [] for e in self.ENGS}
        self.cnt = {e: 0 for e in self.ENGS}
        self.seen = {e: {} for e in self.ENGS}
        self.sem_names = list(self.ENGS)
        self.dma_free = []
        self.dma_assign = {}

    def _dma_key(self, buf):
        k = self.dma_assign.get(id(buf))
        if k is None:
            if self.dma_free:
                k = self.dma_free.pop()
            else:
                k = f"dq{len(self.sem_names)}"
                self.cnt[k] = 0
                self.sem_names.append(k)
            self.dma_assign[id(buf)] = k
        return k

    def _dep(self, eng, reads, writes):
        waits = {}

        def need(k, v):
            if waits.get(k, 0) < v:
                waits[k] = v
        for b in reads:
            b = _b(b)
            if b.w is not None:
                need(*b.w)
        for b in writes:
            b = _b(b)
            if b.w is not None:
                need(*b.w)
            for k, v in b.r.items():
                need(k, v)
        out = []
        seen = self.seen[eng]
        for k, v in waits.items():
            if k == "pe" and eng == "pe":
                continue
            if seen.get(k, 0) >= v:
                continue
            seen[k] = v
            out.append((k, v))
        return out

    def _mark(self, tok, reads, writes):
        k, v = tok
        for b in reads:
            _b(b).r[k] = v
        for b in writes:
            b = _b(b)
            b.w = tok
            b.r = {}

    def op(self, eng, name, *args, reads=(), writes=(), **kw):
        fn = (name, args, kw)
        waits = self._dep(eng, reads, writes)
        if eng == "pe" and name == "matmul" and kw.get("stop") is False:
            tok = (eng, self.cnt[eng] + 1)
            self.ops[eng].append((waits, fn, None))
        else:
            self.cnt[eng] += 1
            tok = (eng, self.cnt[eng])
            self.ops[eng].append((waits, fn, (eng, 1)))
        self._mark(tok, reads, writes)

    def dma(self, queue, out, in_, reads=(), writes=(), stream="d0"):
        fn = ("dma_start", (), {"out": out, "in_": in_})
        prim = _b(writes[0]) if len(writes) else _b(reads[0])
        key = self._dma_key(prim)
        waits = self._dep(queue, reads, writes)
        self.cnt[key] += 16
        tok = (key, self.cnt[key])
        self.ops[queue].append((waits, fn, (key, 16)))
        self._mark(tok, reads, writes)

    def barrier(self, engs=None):
        for e in (engs or self.ENGS):
            waits = []
            for k, v in self.cnt.items():
                if v > 0 and self.seen[e].get(k, 0) < v:
                    self.seen[e][k] = v
                    waits.append((k, v))
            self.ops[e].append((waits, None, None))
        self.dma_free.extend(self.dma_assign.values())
        self.dma_assign.clear()

    def simulate(self):
        cnt = {k: 0 for k in self.cnt}
        ptr = {e: 0 for e in self.ENGS}
        progress = True
        while progress:
            progress = False
            for e in self.ENGS:
                ops = self.ops[e]
                while ptr[e] < len(ops):
                    waits, fn, inc = ops[ptr[e]]
                    if any(cnt[k] < v for k, v in waits):
                        break
                    if inc is not None:
                        cnt[inc[0]] += inc[1]
                    ptr[e] += 1
                    progress = True
        stuck = {e: (ptr[e], len(self.ops[e])) for e in self.ENGS if ptr[e] < len(self.ops[e])}
        if stuck:
            msg = []
            for e, (p_, n_) in stuck.items():
                waits, fn, inc = self.ops[e][p_]
                msg.append(f"{e}@{p_}/{n_} op={fn[0] if fn else None} waits={[(k, v, cnt[k]) for k, v in waits]}")
            raise RuntimeError("semaphore deadlock: " + " | ".join(msg))

    def finish(self):
        nc = self.nc
        self.barrier()
        self.simulate()
        with contextlib.ExitStack() as st:
            sems = {n: st.enter_context(nc.semaphore(n)) for n in self.sem_names}
            block = st.enter_context(nc.Block())

            def emit(engname):
                def f(e):
                    for waits, fn, inc in self.ops[engname]:
                        for k, v in waits:
                            e.wait_ge(sems[k], v)
                        if fn is not None:
                            ins = getattr(e, fn[0])(*fn[1], **fn[2])
                            if inc is not None:
                                ins.then_inc(sems[inc[0]], inc[1])
                return f
            block.tensor(emit("pe"))
            block.scalar(emit("act"))
            block.vector(emit("dve"))
            block.gpsimd(emit("pool"))
            block.sync(emit("sp"))


NPASS = 4
PTOK = TOK // NPASS
NBIS = 34


def build_moe():
    nc = bass.Bass("TRN2", target_bir_lowering=False)
    affT_d = nc.dram_tensor("aff_full", [128, 128 * NEXP], F32, kind="ExternalInput").ap()
    affo_d = nc.dram_tensor("aff_own", [128, 32 * NEXP], F32, kind="ExternalInput").ap()
    hn2_d = nc.dram_tensor("hn2T", [128, 8, TOK], BF16, kind="ExternalInput").ap()
    xmid_d = nc.dram_tensor("xmid", [128, 32, D], F32, kind="ExternalInput").ap()
    wg_d = nc.dram_tensor("wg", [NEXP, D, D], F32, kind="ExternalInput").ap()
    wu_d = nc.dram_tensor("wu", [NEXP, D, D], F32, kind="ExternalInput").ap()
    wd_d = nc.dram_tensor("wd", [NEXP, D, D], F32, kind="ExternalInput").ap()
    xout_d = nc.dram_tensor("xout", [128, 32, D], F32, kind="ExternalOutput").ap()
    S = Sched(nc)

    affT = Tl(nc, "affT", [128, 128, NEXP], F32)
    cmp_ = Tl(nc, "cmp", [128, 128, NEXP], F32)
    affo = Tl(nc, "affo", [128, 32, NEXP], F32)
    cw = Tl(nc, "cw", [128, 32, NEXP], F32)
    ones_f = Tl(nc, "ones_f", [128, 128], F32)
    small = {n: Tl(nc, n, [128, NEXP], F32) for n in ("lo", "hi", "mid", "sel", "t1", "t2", "cntp")}
    cnt_ps = Tl(nc, "cnt_ps", [128, 512], F32, psum=True)

    S.dma("sp", out=affT[:].rearrange("p j e -> p (j e)"), in_=affT_d, writes=[affT], stream="ld")
    S.dma("sp", out=affo[:].rearrange("p j e -> p (j e)"), in_=affo_d, writes=[affo], stream="ld")
    S.op("dve", "memset", ones_f[:], 1.0, writes=[ones_f])
    S.op("dve", "memset", small["lo"][:], 0.0, writes=[small["lo"]])
    S.op("dve", "memset", small["hi"][:], 1.0, writes=[small["hi"]])
    S.op("dve", "memset", small["mid"][:], 0.5, writes=[small["mid"]])
    lo, hi, mid, sel, t1, t2, cntp = (small[n] for n in ("lo", "hi", "mid", "sel", "t1", "t2", "cntp"))
    for it in range(NBIS):
        S.op("dve", "tensor_tensor", out=cmp_[:], in0=affT[:],
                                              in1=mid[:].unsqueeze(1).to_broadcast([128, 128, NEXP]), op=ALU.is_ge,
             reads=[affT, mid], writes=[cmp_])
        S.op("dve", "tensor_reduce", out=cntp[:], in_=cmp_[:].rearrange("p j e -> p e j"), axis=AX.X, op=ALU.add,
             reads=[cmp_], writes=[cntp])
        S.op("pe", "matmul", cnt_ps[:, 0:NEXP], ones_f[:], cntp[:], start=True, stop=True,
             reads=[ones_f, cntp], writes=[cnt_ps])
        S.op("dve", "tensor_scalar", out=sel[:], in0=cnt_ps[:, 0:NEXP], scalar1=float(CAP) - 0.5, scalar2=None, op0=ALU.is_ge,
             reads=[cnt_ps], writes=[sel])
        S.op("dve", "tensor_tensor", out=t1[:], in0=sel[:], in1=mid[:], op=ALU.mult, reads=[sel, mid], writes=[t1])
        S.op("dve", "tensor_tensor", out=lo[:], in0=lo[:], in1=t1[:], op=ALU.max, reads=[lo, t1], writes=[lo])
        S.op("dve", "scalar_tensor_tensor", out=t2[:], in0=sel[:], scalar=4.0, in1=mid[:], op0=ALU.mult, op1=ALU.add,
             reads=[sel, mid], writes=[t2])
        S.op("dve", "tensor_tensor", out=hi[:], in0=hi[:], in1=t2[:], op=ALU.min, reads=[hi, t2], writes=[hi])
        S.op("dve", "tensor_tensor", out=t1[:], in0=lo[:], in1=hi[:], op=ALU.add, reads=[lo, hi], writes=[t1])
        S.op("dve", "tensor_scalar", out=mid[:], in0=t1[:], scalar1=0.5, scalar2=None, op0=ALU.mult, reads=[t1], writes=[mid])
    S.op("dve", "tensor_tensor", out=cw[:], in0=affo[:], in1=lo[:].unsqueeze(1).to_broadcast([128, 32, NEXP]), op=ALU.is_ge,
         reads=[affo, lo], writes=[cw])
    S.op("dve", "tensor_tensor", out=cw[:], in0=cw[:], in1=affo[:], op=ALU.mult, reads=[cw, affo], writes=[cw])

    NW = 4
    wring = [Tl(nc, f"w{i}", [128, 8, D], BF16) for i in range(NW)]
    hn2 = [Tl(nc, f"hn2_{i}", [128, 8, PTOK], BF16) for i in range(2)]
    acc = [Tl(nc, f"acc{i}", [128, PTOK // 128, D], F32) for i in range(1)]
    H = Tl(nc, "H", [128, 8, PTOK], BF16)
    sgt = [Tl(nc, f"sg{i}", [128, 512], F32) for i in range(2)]
    pg = [Tl(nc, f"pg{i}", [128, 512], F32, psum=True) for i in range(2)]
    pu = [Tl(nc, f"pu{i}", [128, 512], F32, psum=True) for i in range(2)]
    py = [Tl(nc, f"py{i}", [128, 512], F32, psum=True) for i in range(2)]

    wseq = []
    for ps in range(NPASS):
        for ex in range(NEXP):
            wseq += [(wg_d, ex), (wu_d, ex), (wd_d, ex)]
    wstate = {"next": 0}

    def issue_w():
        i = wstate["next"]
        if i >= len(wseq):
            return
        wstate["next"] += 1
        src, ex = wseq[i]
        dst = wring[i % NW]
        S.dma("pool", out=dst[:], in_=src[ex].rearrange("(c p) f -> p c f", p=128),
              writes=[dst], stream="w")

    for _ in range(NW - 1):
        issue_w()
    wi = 0
    for ps in range(NPASS):
        hb = hn2[ps % 2]
        ac = acc[0]
        S.dma("sp", out=hb[:], in_=hn2_d[:, :, ps * PTOK:(ps + 1) * PTOK], writes=[hb], stream="ld")
        S.dma("sp", out=ac[:], in_=xmid_d[:, ps * 8:(ps + 1) * 8, :], writes=[ac], stream="ld")
        for ex in range(NEXP):
            G = wring[wi % NW]
            U = wring[(wi + 1) % NW]
            Dn = wring[(wi + 2) % NW]
            wi += 3
            issue_w()
            for blk in range(PTOK // 512):
                ts = slice(blk * 512, (blk + 1) * 512)
                for f in range(8):
                    g_, u_, s_ = pg[f % 2], pu[f % 2], sgt[f % 2]
                    fs = slice(f * 128, (f + 1) * 128)
                    for k in range(8):
                        S.op("pe", "matmul", g_[:], G[:, k, fs], hb[:, k, ts], start=(k == 0), stop=(k == 7),
                             reads=[G, hb], writes=[g_])
                    for k in range(8):
                        S.op("pe", "matmul", u_[:], U[:, k, fs], hb[:, k, ts], start=(k == 0), stop=(k == 7),
                             reads=[U, hb], writes=[u_])
                    S.op("act", "activation", out=s_[:], in_=g_[:], func=AF.Silu, reads=[g_], writes=[s_])
                    S.op("dve", "tensor_tensor", out=H[:, f, ts], in0=s_[:], in1=u_[:], op=ALU.mult,
                         reads=[s_, u_], writes=[H])
            issue_w()
            issue_w()
            for t in range(PTOK // 128):
                for dh in range(2):
                    y_ = py[(t * 2 + dh) % 2]
                    ds_ = slice(dh * 512, (dh + 1) * 512)
                    for f in range(8):
                        S.op("pe", "matmul", y_[:], H[:, f, t * 128:(t + 1) * 128], Dn[:, f, ds_], start=(f == 0), stop=(f == 7),
                             reads=[H, Dn], writes=[y_])
                    tg = ps * 8 + t
                    S.op("dve", "scalar_tensor_tensor", out=ac[:, t, ds_], in0=y_[:], scalar=cw[:, tg, ex:ex + 1],
                                                                 in1=ac[:, t, ds_], op0=ALU.mult, op1=ALU.add,
                         reads=[y_, cw, ac], writes=[ac])
        S.dma("sp", out=xout_d[:, ps * 8:(ps + 1) * 8, :], in_=ac[:], reads=[ac], stream="st")
    S.finish()
    return nc


EXT = TOK + 1024
NQB = TOK // 512
import os
LAG = int(os.environ.get("MIX_LAG", "2"))
C_QA, C_KA, C_VA, C_CQ, C_CKV, C_KR, C_HC, C_BC, C_CC = 0, 384, 768, 1152, 1408, 1536, 1568, 1824, 2080


_UID = [0]


def _uq(name):
    _UID[0] += 1
    return f"{name}_u{_UID[0]}"


class Scope:
    def __init__(self, nc):
        self.nc = nc
        self.st = contextlib.ExitStack()

    def sb(self, name, shape, dtype):
        t = Tl.__new__(Tl)
        t.t = self.st.enter_context(self.nc.sbuf_tensor(_uq(name), shape, dtype))
        t.b = Buf(name)
        return t

    def ps(self, name, shape=(128, 512), dtype=F32):
        t = Tl.__new__(Tl)
        t.t = self.st.enter_context(self.nc.psum_tensor(_uq(name), list(shape), dtype))
        t.b = Buf(name)
        return t

    def close(self):
        self.st.close()


def build_mix(debug=False):
    nc = bass.Bass("TRN2", target_bir_lowering=False)

    def din(name, shape, dt=F32):
        return nc.dram_tensor(name, list(shape), dt, kind="ExternalInput").ap()

    def dscr(name, shape, dt, out=False):
        return nc.dram_tensor(name, list(shape), dt, kind="ExternalOutput" if (out or debug) else "Internal").ap()

    xTf_d = din("xT_full", [128, 8, S_LEN])
    xTe_d = din("xT_ext", [128, 8, EXT])
    w_in_d = din("w_in", [D, 2336])
    w_uq_d = din("w_uq", [256, 576])
    w_ukv_d = din("w_ukv", [128, 768])
    w_out_d = din("w_out", [D, D])
    w_rt_d = din("w_router", [D, NEXP])
    g_mix_d = din("g_mix", [128, 8])
    g_qa_d = din("g_qa", [128, 1])
    g_ka_d = din("g_ka", [128, 1])
    g_cq_d = din("g_cq", [128, 2])
    g_ckv_d = din("g_ckv", [128, 1])
    g_qb_d = din("g_qb", [96, 1])
    g_kb_d = din("g_kb", [96, 1])
    convw_d = din("conv_w", [128, 2, 3])
    g_oa_d = din("g_oa", [64, 6])
    g_ob_d = din("g_ob", [64, 6])
    g_oc_d = din("g_oc", [128, 2])
    g_ffn_d = din("g_ffn", [128, 8])
    b_rt_d = din("b_router", [NEXP, 1])
    ropeKC_d = din("ropeKC", [96, S_LEN])
    ropeKS_d = din("ropeKS", [96, S_LEN])
    ropeQC_d = din("ropeQC", [96, TOK])
    ropeQS_d = din("ropeQS", [96, TOK])
    rot_d = din("rotT", [96, 96])
    bd_d = din("blockdiag", [128, 128])
    ident_d = din("ident", [128, 128])
    biasA_d = din("biasA", [3, 6, 128, 8, 512])

    xmid_o = dscr("xmidT", [128, 8, TOK], F32, out=True)
    hn2_o = dscr("hn2T", [128, 8, TOK], BF16, out=True)
    aff_o = dscr("affT", [NEXP, TOK], F32, out=True)
    KbT_d = dscr("KbT", [6, 96, S_LEN], BF16)
    Vb_d = dscr("Vb", [6, 128, S_LEN // 128, 128], BF16)
    QbT_d = dscr("QbT", [6, 96, TOK], BF16)
    KaT_d = dscr("KaT", [128, 3, EXT], BF16)
    QaT_d = dscr("QaT", [128, 3, TOK], BF16)
    Va_d = dscr("Va", [6, 128, EXT // 128, 128], BF16)
    U_d = dscr("U", [128, 2, EXT], F32)
    Bc_d = dscr("Bc", [128, 2, TOK], F32)
    mixT_d = dscr("mixT", [D, TOK], F32)
    biasA_bf = dscr("biasA_bf", [3, 6, 128, 8, 512], BF16)

    S = Sched(nc)
    G = Scope(nc)
    ones_bf = G.sb("ones_bf", [128, 128], BF16)
    ones_f = G.sb("ones_f", [128, 128], F32)
    eps_t = G.sb("eps_t", [128, 1], F32)
    bd_bf = G.sb("bd_bf", [128, 128], BF16)
    id_bf = G.sb("id_bf", [128, 128], BF16)
    rot_bf = G.sb("rot_bf", [96, 96], BF16)
    gt = {}
    for name, src, shp in [("g_mix", g_mix_d, [128, 8]), ("g_qa", g_qa_d, [128, 1]), ("g_ka", g_ka_d, [128, 1]),
                           ("g_cq", g_cq_d, [128, 2]), ("g_ckv", g_ckv_d, [128, 1]), ("g_qb", g_qb_d, [96, 1]),
                           ("g_kb", g_kb_d, [96, 1]), ("g_oa", g_oa_d, [64, 6]), ("g_ob", g_ob_d, [64, 6]),
                           ("g_oc", g_oc_d, [128, 2]), ("g_ffn", g_ffn_d, [128, 8]), ("b_rt", b_rt_d, [NEXP, 1])]:
        gt[name] = G.sb(name, shp, F32)
        S.dma("sp", out=gt[name][:], in_=src, writes=[gt[name]], stream="ld")
    convw = G.sb("convw", [128, 2, 3], F32)
    S.dma("sp", out=convw[:], in_=convw_d, writes=[convw], stream="ld")
    S.op("dve", "memset", ones_bf[:], 1.0, writes=[ones_bf])
    S.op("dve", "memset", ones_f[:], 1.0, writes=[ones_f])
    S.op("dve", "memset", eps_t[:], EPS, writes=[eps_t])
    S.dma("pool", out=bd_bf[:], in_=bd_d, writes=[bd_bf], stream="ldc")
    S.dma("pool", out=id_bf[:], in_=ident_d, writes=[id_bf], stream="ldc")
    S.dma("pool", out=rot_bf[:], in_=rot_d, writes=[rot_bf], stream="ldc")
    bstg = [G.sb(f"bstg{i}", [128, 8, 512], BF16) for i in range(2)]
    for var in range(3):
        for h in range(6):
            bs = bstg[(var * 6 + h) % 2]
            S.dma("pool", out=bs[:], in_=biasA_d[var, h], writes=[bs], stream="ldb")
            S.dma("sp", out=biasA_bf[var, h], in_=bs[:], reads=[bs], stream="stb")
    S.op("dve", "tensor_scalar", out=gt["g_qa"][:], in0=gt["g_qa"][:], scalar1=0.125, scalar2=None, op0=ALU.mult,
         reads=[gt["g_qa"]], writes=[gt["g_qa"]])

    def rstd(ps_ap, out_ap, n, parts, rd, wr):
        S.op("act", "activation", out=out_ap, in_=ps_ap, func=AF.Sqrt, scale=1.0 / n, bias=eps_t[0:parts, :],
             reads=[rd, eps_t], writes=[wr])
        S.op("dve", "reciprocal", out=out_ap, in_=out_ap, reads=[wr], writes=[wr])

    def xprep(P, src_d, t0, xin, xb, xsq, rs_ps, rs_bc):
        S.dma("sp", out=xin[:], in_=src_d[:, :, t0:t0 + 512], writes=[xin], stream="ldx")
        S.op("dve", "tensor_tensor", out=xb[:], in0=xin[:], in1=gt["g_mix"][:].unsqueeze(2).to_broadcast([128, 8, 512]),
             op=ALU.mult, reads=[xin, gt["g_mix"]], writes=[xb])
        S.op("act", "activation", out=xsq[:], in_=xin[:], func=AF.Square, reads=[xin], writes=[xsq])
        for k in range(8):
            S.op("pe", "matmul", rs_ps[:], ones_bf[:], xsq[:, k, :], start=(k == 0), stop=(k == 7),
                 reads=[ones_bf, xsq], writes=[rs_ps])
        rstd(rs_ps[:], rs_bc[:], float(D), 128, rs_ps, rs_bc)

    def proj(ps, wt, c0, ncols, xb, out_parts=None):
        for k in range(8):
            S.op("pe", "matmul", ps[0:ncols, :] if out_parts is None else ps[out_parts, :], wt[:, k, c0:c0 + ncols], xb[:, k, :],
                 start=(k == 0), stop=(k == 7), reads=[wt, xb], writes=[ps])

    def norm_rope(P, raw, g, Ct, St, outt, sq, ss_ps, rs, kbn, rot_ps, t1, t2):
        S.op("act", "activation", out=sq[:], in_=raw[:], func=AF.Square, reads=[raw], writes=[sq])
        S.op("pe", "matmul", ss_ps[0:96, :], ones_bf[0:96, 0:96], sq[:], start=True, stop=True, reads=[ones_bf, sq], writes=[ss_ps])
        rstd(ss_ps[0:96, :], rs[:], 96.0, 96, ss_ps, rs)
        S.op("dve", "scalar_tensor_tensor", out=kbn[:], in0=raw[:], scalar=g[:, 0:1], in1=rs[:], op0=ALU.mult, op1=ALU.mult,
             reads=[raw, g, rs], writes=[kbn])
        S.op("pe", "matmul", rot_ps[0:96, :], rot_bf[:], kbn[:], start=True, stop=True, reads=[rot_bf, kbn], writes=[rot_ps])
        S.op("pool", "tensor_tensor", out=t1[:], in0=kbn[:], in1=Ct[:], op=ALU.mult, reads=[kbn, Ct], writes=[t1])
        S.op("dve", "tensor_tensor", out=t2[:], in0=rot_ps[0:96, :], in1=St[:], op=ALU.mult, reads=[rot_ps, St], writes=[t2])
        S.op("dve", "tensor_tensor", out=outt[:], in0=t1[:], in1=t2[:], op=ALU.add, reads=[t1, t2], writes=[outt])

    def head_norm64(ps, rs_bc, g, outt, tf, sq, ss_ps, rs):
        S.op("dve", "tensor_tensor", out=tf[:], in0=ps[:], in1=rs_bc[:], op=ALU.mult, reads=[ps, rs_bc], writes=[tf])
        S.op("act", "activation", out=sq[:], in_=tf[:], func=AF.Square, reads=[tf], writes=[sq])
        S.op("pe", "matmul", ss_ps[:], bd_bf[:], sq[:], start=True, stop=True, reads=[bd_bf, sq], writes=[ss_ps])
        rstd(ss_ps[:], rs[:], 64.0, 128, ss_ps, rs)
        S.op("dve", "scalar_tensor_tensor", out=outt[:], in0=tf[:], scalar=g[:, 0:1], in1=rs[:], op0=ALU.mult, op1=ALU.mult,
             reads=[tf, g, rs], writes=[outt])

    P = Scope(nc)
    wA = P.sb("wA_ckv", [128, 8, 128], BF16)
    wKr = P.sb("wA_kr", [128, 8, 96], BF16)
    wk = P.sb("wukv_k", [128, 6, 64], BF16)
    wv = P.sb("wukv_v", [128, 6, 64], BF16)
    S.dma("pool", out=wA[:], in_=w_in_d[:, C_CKV:C_CKV + 128].rearrange("(c p) f -> p c f", p=128), writes=[wA], stream="ldc")
    S.op("dve", "memset", wKr[:], 0.0, writes=[wKr])
    S.dma("pool", out=wKr[:, :, 64:96], in_=w_in_d[:, C_KR:C_KR + 32].rearrange("(c p) f -> p c f", p=128), writes=[wKr], stream="ldc")
    ukv3 = w_ukv_d.rearrange("r (h x) -> r h x", x=128)
    S.dma("pool", out=wk[:], in_=ukv3[:, :, 0:64], writes=[wk], stream="ldc")
    S.dma("pool", out=wv[:], in_=ukv3[:, :, 64:128], writes=[wv], stream="ldc")
    xin = [P.sb(f"xin{i}", [128, 8, 512], F32) for i in range(2)]
    xb = [P.sb(f"xb{i}", [128, 8, 512], BF16) for i in range(2)]
    xsq = [P.sb(f"xsq{i}", [128, 8, 512], BF16) for i in range(2)]
    rs_bc = [P.sb(f"rsbc{i}", [128, 512], F32) for i in range(2)]
    ckv = P.sb("ckv", [128, 512], F32)
    sqc = P.sb("sqc", [128, 512], BF16)
    rsc = P.sb("rsc", [128, 512], F32)
    cn = [P.sb(f"cn{i}", [128, 512], BF16) for i in range(2)]
    raw = [P.sb(f"raw{i}", [96, 512], F32) for i in range(2)]
    Ct = [P.sb(f"Ct{i}", [96, 512], F32) for i in range(2)]
    St = [P.sb(f"St{i}", [96, 512], F32) for i in range(2)]
    sq96 = P.sb("sq96", [96, 512], BF16)
    rs96 = P.sb("rs96", [96, 512], F32)
    kbn = P.sb("kbn", [96, 512], BF16)
    t1 = P.sb("t1", [96, 512], F32)
    t2 = P.sb("t2", [96, 512], F32)
    ko = [P.sb(f"ko{i}", [96, 512], BF16) for i in range(2)]
    Vst = [P.sb(f"Vst{i}", [128, 6, 4, 128], BF16) for i in range(2)]
    rs_ps = P.ps("rs_ps")
    pj_ps = [P.ps(f"pj_ps{i}") for i in range(2)]
    ss_ps = P.ps("ss_ps")
    rot_ps = P.ps("rot_ps")
    v_ps = [P.ps(f"v_ps{i}") for i in range(2)]
    for i in range(2):
        S.op("dve", "memset", Vst[i][:], 1.0, writes=[Vst[i]])
    for blk in range(S_LEN // 512):
        i = blk % 2
        t0 = blk * 512
        xprep(P, xTf_d, t0, xin[i], xb[i], xsq[i], rs_ps, rs_bc[i])
        S.dma("sp", out=Ct[i][:], in_=ropeKC_d[:, t0:t0 + 512], writes=[Ct[i]], stream="ldx")
        S.dma("sp", out=St[i][:], in_=ropeKS_d[:, t0:t0 + 512], writes=[St[i]], stream="ldx")
        proj(pj_ps[0], wA, 0, 128, xb[i])
        S.op("dve", "tensor_tensor", out=ckv[:], in0=pj_ps[0][:], in1=rs_bc[i][:], op=ALU.mult, reads=[pj_ps[0], rs_bc[i]], writes=[ckv])
        proj(pj_ps[1], wKr, 0, 96, xb[i])
        for r in range(2):
            S.op("dve", "tensor_tensor", out=raw[r][64:96, :], in0=pj_ps[1][64:96, :], in1=rs_bc[i][64:96, :], op=ALU.mult,
                 reads=[pj_ps[1], rs_bc[i]], writes=[raw[r]])
        S.op("act", "activation", out=sqc[:], in_=ckv[:], func=AF.Square, reads=[ckv], writes=[sqc])
        S.op("pe", "matmul", ss_ps[:], ones_bf[:], sqc[:], start=True, stop=True, reads=[ones_bf, sqc], writes=[ss_ps])
        rstd(ss_ps[:], rsc[:], 128.0, 128, ss_ps, rsc)
        S.op("dve", "scalar_tensor_tensor", out=cn[i][:], in0=ckv[:], scalar=gt["g_ckv"][:, 0:1], in1=rsc[:], op0=ALU.mult, op1=ALU.mult,
             reads=[ckv, gt["g_ckv"], rsc], writes=[cn[i]])
        for sub in range(4):
            vp = v_ps[sub % 2]
            S.op("pe", "matmul", vp[:, 0:384], cn[i][:, sub * 128:(sub + 1) * 128], wv[:].rearrange("p h x -> p (h x)"),
                 start=True, stop=True, reads=[cn[i], wv], writes=[vp])
            S.op("act", "activation", out=Vst[i][:, :, sub, 0:64], in_=vp[:, 0:384].rearrange("p (h x) -> p h x", x=64), func=AF.Copy,
                 reads=[vp], writes=[Vst[i]])
        S.dma("pool", out=Vb_d[:, :, blk * 4:(blk + 1) * 4, :].rearrange("h p c x -> p h c x"), in_=Vst[i][:], reads=[Vst[i]], stream="st")
        for h in range(6):
            r = h % 2
            S.op("pe", "matmul", pj_ps[r][0:64, :], wk[:, h, :], cn[i][:], start=True, stop=True, reads=[wk, cn[i]], writes=[pj_ps[r]])
            S.op("act", "activation", out=raw[r][0:64, :], in_=pj_ps[r][0:64, :], func=AF.Copy, reads=[pj_ps[r]], writes=[raw[r]])
            norm_rope(P, raw[r], gt["g_kb"], Ct[i], St[i], ko[r], sq96, ss_ps, rs96, kbn, rot_ps, t1, t2)
            S.dma("pool", out=KbT_d[h, :, t0:t0 + 512], in_=ko[r][:], reads=[ko[r]], stream="st")
    S.barrier()
    P.close()

    P = Scope(nc)
    wB = P.sb("wB", [128, 8, 2336], BF16)
    S.dma("pool", out=wB[:], in_=w_in_d.rearrange("(c p) f -> p c f", p=128), writes=[wB], stream="ldc")
    wuq = P.sb("wuq", [128, 2, 576], BF16)
    S.dma("pool", out=wuq[:], in_=w_uq_d.rearrange("(c p) f -> p c f", p=128), writes=[wuq], stream="ldc")
    xin = [P.sb(f"xin{i}", [128, 8, 512], F32) for i in range(2)]
    xb = [P.sb(f"xb{i}", [128, 8, 512], BF16) for i in range(2)]
    xsq = [P.sb(f"xsq{i}", [128, 8, 512], BF16) for i in range(2)]
    rs_bc = [P.sb(f"rsbc{i}", [128, 512], F32) for i in range(2)]
    tf = P.sb("tf", [128, 512], F32)
    sq = P.sb("sq", [128, 512], BF16)
    rs = P.sb("rs", [128, 512], F32)
    hout = [P.sb(f"hout{i}", [128, 512], BF16) for i in range(2)]
    rtm = P.sb("rtm", [128, 4], F32)
    Vst = [P.sb(f"Vst{i}", [128, 6, 4, 128], BF16) for i in range(2)]
    th = P.sb("th", [128, 512], F32)
    tc_ = P.sb("tc", [128, 512], F32)
    uo = [P.sb(f"uo{i}", [128, 512], F32) for i in range(2)]
    cq = P.sb("cq", [128, 2, 512], F32)
    sq2 = P.sb("sq2", [128, 2, 512], BF16)
    cqn = P.sb("cqn", [128, 2, 512], BF16)
    raw = [P.sb(f"raw{i}", [96, 512], F32) for i in range(2)]
    Ct = P.sb("Ct", [96, 512], F32)
    St = P.sb("St", [96, 512], F32)
    sq96 = P.sb("sq96", [96, 512], BF16)
    rs96 = P.sb("rs96", [96, 512], F32)
    kbn = P.sb("kbn", [96, 512], BF16)
    t1 = P.sb("t1", [96, 512], F32)
    t2 = P.sb("t2", [96, 512], F32)
    ko = [P.sb(f"ko{i}", [96, 512], BF16) for i in range(2)]
    rs_ps = P.ps("rs_ps")
    pj_ps = [P.ps(f"pj_ps{i}") for i in range(2)]
    ss_ps = P.ps("ss_ps")
    rot_ps = P.ps("rot_ps")
    v_ps = [P.ps(f"v_ps{i}") for i in range(2)]
    tm_ps = P.ps("tm_ps")
    for i in range(2):
        S.op("dve", "memset", Vst[i][:], 1.0, writes=[Vst[i]])
    npj = [0]

    def nextps():
        npj[0] += 1
        return pj_ps[npj[0] % 2]
    nh = [0]

    def nexth():
        nh[0] += 1
        return hout[nh[0] % 2]
    for blk in range(EXT // 512):
        i = blk % 2
        t0 = blk * 512
        own = 1 <= blk <= NQB
        q0 = t0 - 512
        xprep(P, xTe_d, t0, xin[i], xb[i], xsq[i], rs_ps, rs_bc[i])
        for p in range(3):
            ps = nextps()
            proj(ps, wB, C_KA + p * 128, 128, xb[i])
            ho = nexth()
            head_norm64(ps, rs_bc[i], gt["g_ka"], ho, tf, sq, ss_ps, rs)
            S.dma("pool", out=KaT_d[:, p, t0:t0 + 512], in_=ho[:], reads=[ho], stream="st")
        for sub in range(4):
            for k in range(8):
                S.op("pe", "matmul", tm_ps[:, sub:sub + 1], xsq[i][:, k, sub * 128:(sub + 1) * 128], ones_bf[:, 0:1],
                     start=(k == 0), stop=(k == 7), reads=[xsq[i], ones_bf], writes=[tm_ps])
        rstd(tm_ps[:, 0:4], rtm[:], float(D), 128, tm_ps, rtm)
        for sub in range(4):
            vp = v_ps[sub % 2]
            for k in range(8):
                S.op("pe", "matmul", vp[:, 0:384], xb[i][:, k, sub * 128:(sub + 1) * 128], wB[:, k, C_VA:C_VA + 384],
                     start=(k == 0), stop=(k == 7), reads=[xb[i], wB], writes=[vp])
            S.op("act", "activation", out=Vst[i][:, :, sub, 0:64], in_=vp[:, 0:384].rearrange("p (h x) -> p h x", x=64), func=AF.Copy,
                 scale=rtm[:, sub:sub + 1], reads=[vp, rtm], writes=[Vst[i]])
        S.dma("pool", out=Va_d[:, :, blk * 4:(blk + 1) * 4, :].rearrange("h p c x -> p h c x"), in_=Vst[i][:], reads=[Vst[i]], stream="st")
        for c in range(2):
            ph = nextps()
            proj(ph, wB, C_HC + c * 128, 128, xb[i])
            S.op("dve", "tensor_tensor", out=th[:], in0=ph[:], in1=rs_bc[i][:], op=ALU.mult, reads=[ph, rs_bc[i]], writes=[th])
            pc = nextps()
            proj(pc, wB, C_CC + c * 128, 128, xb[i])
            S.op("dve", "tensor_tensor", out=tc_[:], in0=pc[:], in1=rs_bc[i][:], op=ALU.mult, reads=[pc, rs_bc[i]], writes=[tc_])
            u_ = uo[c]
            S.op("pool", "tensor_tensor", out=u_[:], in0=th[:], in1=tc_[:], op=ALU.mult, reads=[th, tc_], writes=[u_])
            S.dma("pool", out=U_d[:, c, t0:t0 + 512], in_=u_[:], reads=[u_], stream="st")
        if not own:
            continue
        for p in range(3):
            ps = nextps()
            proj(ps, wB, C_QA + p * 128, 128, xb[i])
            ho = nexth()
            head_norm64(ps, rs_bc[i], gt["g_qa"], ho, tf, sq, ss_ps, rs)
            S.dma("pool", out=QaT_d[:, p, q0:q0 + 512], in_=ho[:], reads=[ho], stream="st")
        for c in range(2):
            ps = nextps()
            proj(ps, wB, C_BC + c * 128, 128, xb[i])
            u_ = uo[c]
            S.op("dve", "tensor_tensor", out=u_[:], in0=ps[:], in1=rs_bc[i][:], op=ALU.mult, reads=[ps, rs_bc[i]], writes=[u_])
            S.dma("pool", out=Bc_d[:, c, q0:q0 + 512], in_=u_[:], reads=[u_], stream="st")
        for c in range(2):
            ps = nextps()
            proj(ps, wB, C_CQ + c * 128, 128, xb[i])
            S.op("dve", "tensor_tensor", out=cq[:, c, :], in0=ps[:], in1=rs_bc[i][:], op=ALU.mult, reads=[ps, rs_bc[i]], writes=[cq])
        S.op("act", "activation", out=sq2[:], in_=cq[:], func=AF.Square, reads=[cq], writes=[sq2])
        for c in range(2):
            S.op("pe", "matmul", ss_ps[:], ones_bf[:], sq2[:, c, :], start=(c == 0), stop=(c == 1), reads=[ones_bf, sq2], writes=[ss_ps])
        rstd(ss_ps[:], rs[:], 256.0, 128, ss_ps, rs)
        for c in range(2):
            S.op("dve", "scalar_tensor_tensor", out=cqn[:, c, :], in0=cq[:, c, :], scalar=gt["g_cq"][:, c:c + 1], in1=rs[:],
                 op0=ALU.mult, op1=ALU.mult, reads=[cq, gt["g_cq"], rs], writes=[cqn])
        S.dma("sp", out=Ct[:], in_=ropeQC_d[:, q0:q0 + 512], writes=[Ct], stream="ldx")
        S.dma("sp", out=St[:], in_=ropeQS_d[:, q0:q0 + 512], writes=[St], stream="ldx")
        for h in range(6):
            r = h % 2
            ps = nextps()
            for c in range(2):
                S.op("pe", "matmul", ps[0:96, :], wuq[:, c, h * 96:(h + 1) * 96], cqn[:, c, :], start=(c == 0), stop=(c == 1),
                     reads=[wuq, cqn], writes=[ps])
            S.op("act", "activation", out=raw[r][:], in_=ps[0:96, :], func=AF.Copy, reads=[ps], writes=[raw[r]])
            norm_rope(P, raw[r], gt["g_qb"], Ct, St, ko[r], sq96, ss_ps, rs96, kbn, rot_ps, t1, t2)
            S.dma("pool", out=QbT_d[h, :, q0:q0 + 512], in_=ko[r][:], reads=[ko[r]], stream="st")
    S.barrier()
    P.close()

    def finalize(po, rec, on, dst_ap):
        S.op("dve", "reciprocal", out=rec[64:128, :], in_=po[64:128, :], reads=[po], writes=[rec])
        S.op("dve", "tensor_tensor", out=on[:], in0=po[0:64, :], in1=rec[64:128, :], op=ALU.mult, reads=[po, rec], writes=[on])
        S.dma("pool", out=dst_ap, in_=on[:], reads=[on], stream="st")

    def run_pipeline(items, lag=LAG):
        for idx in range(len(items) + lag):
            if idx < len(items):
                items[idx][0]()
            if idx >= lag:
                items[idx - lag][1]()

    P = Scope(nc)
    Kt = [P.sb(f"Kt{i}", [128, 1024], BF16) for i in range(2)]
    Qt = [P.sb(f"Qt{i}", [128, 512], BF16) for i in range(2)]
    Vt = [P.sb(f"Vt{i}", [128, 8, 128], BF16) for i in range(3)]
    Bt = [P.sb(f"Bt{i}", [128, 8, 512], BF16) for i in range(3)]
    Pt = [P.sb(f"Pt{i}", [128, 512], BF16) for i in range(4)]
    rec = P.sb("rec", [128, 512], F32)
    on = [P.sb(f"on{i}", [64, 512], F32) for i in range(2)]
    s_ps = [P.ps(f"s_ps{i}") for i in range(4)]
    o_ps = [P.ps(f"o_ps{i}") for i in range(2)]
    items = []
    n = 0
    it = 0
    for j in range(NQB):
        var = 0 if j == 0 else (2 if j == NQB - 1 else 1)
        k0 = 512 * j + 256
        for p in range(3):
            kt, qt = Kt[(j * 3 + p) % 2], Qt[(j * 3 + p) % 2]
            for hh in range(2):
                h = p * 2 + hh
                vt, bt, po, on_ = Vt[it % 3], Bt[it % 3], o_ps[it % 2], on[it % 2]
                it += 1
                pp = slice(hh * 64, (hh + 1) * 64)
                for c in range(8):
                    sp_, pt = s_ps[n % 4], Pt[n % 4]
                    n += 1

                    def s_fn(j=j, p=p, hh=hh, h=h, c=c, kt=kt, qt=qt, vt=vt, bt=bt, sp_=sp_, pt=pt, pp=pp, k0=k0, var=var):
                        if c == 0 and hh == 0:
                            S.dma("sp", out=kt[:], in_=KaT_d[:, p, k0:k0 + 1024], writes=[kt], stream="ldx")
                            S.dma("sp", out=qt[:], in_=QaT_d[:, p, j * 512:(j + 1) * 512], writes=[qt], stream="ldx")
                        if c == 0:
                            S.dma("sp", out=vt[:], in_=Va_d[h, :, k0 // 128:k0 // 128 + 8, :], writes=[vt], stream="ldx")
                            S.dma("sp", out=bt[:], in_=biasA_bf[var, h], writes=[bt], stream="ldx")
                        S.op("pe", "matmul", sp_[:], kt[pp, c * 128:(c + 1) * 128], qt[pp, :], start=True, stop=False, reads=[kt, qt], writes=[sp_])
                        S.op("pe", "matmul", sp_[:], id_bf[:], bt[:, c, :], start=False, stop=True, reads=[id_bf, bt], writes=[sp_])
                        S.op("act", "activation", out=pt[:], in_=sp_[:], func=AF.Exp, reads=[sp_], writes=[pt])

                    def pv_fn(j=j, h=h, c=c, vt=vt, pt=pt, po=po, on_=on_):
                        S.op("pe", "matmul", po[:], vt[:, c, :], pt[:], start=(c == 0), stop=(c == 7), reads=[vt, pt], writes=[po])
                        if c == 7:
                            finalize(po, rec, on_, mixT_d[h * 64:(h + 1) * 64, j * 512:(j + 1) * 512])
                    items.append((s_fn, pv_fn))
    run_pipeline(items)
    S.barrier()
    P.close()

    P = Scope(nc)
    Kh = [P.sb(f"Kh{i}", [96, S_LEN], BF16) for i in range(2)]
    Vh = [P.sb(f"Vh{i}", [128, S_LEN // 128, 128], BF16) for i in range(2)]
    Qh = [P.sb(f"Qh{i}", [96, TOK], BF16) for i in range(2)]
    Pt = [P.sb(f"Pt{i}", [128, 512], BF16) for i in range(4)]
    rec = P.sb("rec", [128, 512], F32)
    on = [P.sb(f"on{i}", [64, 512], F32) for i in range(2)]
    s_ps = [P.ps(f"s_ps{i}") for i in range(4)]
    o_ps = [P.ps(f"o_ps{i}") for i in range(2)]
    items = []
    n = 0
    it = 0
    sc_b = 96.0 ** -0.5
    NKC = S_LEN // 128

    def load_head(h):
        kh, vh, qh = Kh[h % 2], Vh[h % 2], Qh[h % 2]
        S.dma("sp", out=kh[:], in_=KbT_d[h], writes=[kh], stream="ldx")
        S.dma("sp", out=vh[:], in_=Vb_d[h], writes=[vh], stream="ldx")
        S.dma("sp", out=qh[:], in_=QbT_d[h], writes=[qh], stream="ldx")
    for h in range(6):
        kh, vh, qh = Kh[h % 2], Vh[h % 2], Qh[h % 2]
        for j in range(NQB):
            po, on_ = o_ps[it % 2], on[it % 2]
            it += 1
            for c in range(NKC):
                sp_, pt = s_ps[n % 4], Pt[n % 4]
                n += 1

                def s_fn(h=h, j=j, c=c, kh=kh, qh=qh, sp_=sp_, pt=pt):
                    if j == 0 and c == 0 and h == 0:
                        load_head(0)
                    if j == 0 and c == 8 and h + 1 < 6:
                        load_head(h + 1)
                    S.op("pe", "matmul", sp_[:], kh[:, c * 128:(c + 1) * 128], qh[:, j * 512:(j + 1) * 512], start=True, stop=True,
                         reads=[kh, qh], writes=[sp_])
                    S.op("act", "activation", out=pt[:], in_=sp_[:], func=AF.Exp, scale=sc_b, reads=[sp_], writes=[pt])

                def pv_fn(h=h, j=j, c=c, vh=vh, pt=pt, po=po, on_=on_):
                    S.op("pe", "matmul", po[:], vh[:, c, :], pt[:], start=(c == 0), stop=(c == NKC - 1), reads=[vh, pt], writes=[po])
                    if c == NKC - 1:
                        finalize(po, rec, on_, mixT_d[384 + h * 64:384 + (h + 1) * 64, j * 512:(j + 1) * 512])
                items.append((s_fn, pv_fn))
    run_pipeline(items)
    S.barrier()
    P.close()

    P = Scope(nc)
    woA = P.sb("woA", [64, 6, D], BF16)
    woB = P.sb("woB", [64, 6, D], BF16)
    woC = P.sb("woC", [128, 2, D], BF16)
    wrt = P.sb("wrt", [128, 8, NEXP], F32)
    S.dma("pool", out=woA[:], in_=w_out_d[0:384, :].rearrange("(h p) d -> p h d", p=64), writes=[woA], stream="ldc")
    S.dma("pool", out=woB[:], in_=w_out_d[384:768, :].rearrange("(h p) d -> p h d", p=64), writes=[woB], stream="ldc")
    S.dma("pool", out=woC[:], in_=w_out_d[768:1024, :].rearrange("(c p) d -> p c d", p=128), writes=[woC], stream="ldc")
    S.dma("sp", out=wrt[:], in_=w_rt_d.rearrange("(c p) e -> p c e", p=128), writes=[wrt], stream="ld")
    ut = [P.sb(f"ut{i}", [128, 2, 514], F32) for i in range(1)]
    bct = [P.sb(f"bct{i}", [128, 2, 512], F32) for i in range(1)]
    yc = P.sb("yc", [128, 2, 512], F32)
    mA = [P.sb(f"mA{i}", [64, 6, 512], F32) for i in range(1)]
    mB = [P.sb(f"mB{i}", [64, 6, 512], F32) for i in range(1)]
    xo = [P.sb(f"xo{i}", [128, 8, 512], F32) for i in range(1)]
    sqA = P.sb("sqA", [64, 6, 512], BF16)
    sqC = P.sb("sqC", [128, 2, 512], BF16)
    rsn = P.sb("rsn", [128, 512], F32)
    tA = P.sb("tA", [64, 6, 512], F32)
    nA = P.sb("nA", [64, 6, 512], BF16)
    nB = P.sb("nB", [64, 6, 512], BF16)
    nC = P.sb("nC", [128, 2, 512], BF16)
    xm = [P.sb(f"xm{i}", [128, 8, 512], F32) for i in range(1)]
    sqx = P.sb("sqx", [128, 8, 512], BF16)
    hf = P.sb("hf", [128, 8, 512], F32)
    hb_ = [P.sb(f"hb{i}", [128, 8, 512], BF16) for i in range(1)]
    ex = P.sb("ex", [NEXP, 512], F32)
    rsum = P.sb("rsum", [NEXP, 512], F32)
    affo = [P.sb(f"affo{i}", [NEXP, 512], F32) for i in range(1)]
    ss_ps = P.ps("ss_ps")
    w_ps = [P.ps(f"w_ps{i}") for i in range(2)]
    r_ps = P.ps("r_ps")
    mAd = mixT_d[0:384, :].rearrange("(h p) t -> p h t", p=64)
    mBd = mixT_d[384:768, :].rearrange("(h p) t -> p h t", p=64)
    mCd = mixT_d[768:1024, :].rearrange("(c p) t -> p c t", p=128)

    def seg_norm(src, nchunk, parts, g, sqt, tmp, outt, n):
        S.op("act", "activation", out=sqt[:], in_=src[:], func=AF.Square, reads=[src], writes=[sqt])
        for c in range(nchunk):
            S.op("pe", "matmul", ss_ps[0:parts, :], ones_bf[0:parts, 0:parts], sqt[:, c, :], start=(c == 0), stop=(c == nchunk - 1),
                 reads=[ones_bf, sqt], writes=[ss_ps])
        rstd(ss_ps[0:parts, :], rsn[0:parts, :], float(n), parts, ss_ps, rsn)
        S.op("dve", "tensor_tensor", out=tmp[:], in0=src[:], in1=g[:].unsqueeze(2).to_broadcast([parts, nchunk, 512]), op=ALU.mult,
             reads=[src, g], writes=[tmp])
        S.op("dve", "tensor_tensor", out=outt[:], in0=tmp[:], in1=rsn[0:parts, :].unsqueeze(1).to_broadcast([parts, nchunk, 512]), op=ALU.mult,
             reads=[tmp, rsn], writes=[outt])

    for j in range(NQB):
        i = 0
        e0 = 512 * (j + 1)
        S.dma("sp", out=ut[i][:], in_=U_d[:, :, e0 - 1:e0 + 513], writes=[ut[i]], stream="ldx")
        S.dma("sp", out=bct[i][:], in_=Bc_d[:, :, j * 512:(j + 1) * 512], writes=[bct[i]], stream="ldx")
        for c in range(2):
            S.op("dve", "tensor_scalar", out=yc[:, c, :], in0=ut[i][:, c, 0:512], scalar1=convw[:, c, 0:1], scalar2=None, op0=ALU.mult,
                 reads=[ut[i], convw], writes=[yc])
            for kk in (1, 2):
                S.op("dve", "scalar_tensor_tensor", out=yc[:, c, :], in0=ut[i][:, c, kk:kk + 512], scalar=convw[:, c, kk:kk + 1], in1=yc[:, c, :],
                     op0=ALU.mult, op1=ALU.add, reads=[ut[i], convw, yc], writes=[yc])
        S.op("dve", "tensor_tensor", out=yc[:], in0=yc[:], in1=bct[i][:], op=ALU.mult, reads=[yc, bct[i]], writes=[yc])
        S.dma("sp", out=mA[i][:], in_=mAd[:, :, j * 512:(j + 1) * 512], writes=[mA[i]], stream="ldx")
        S.dma("sp", out=mB[i][:], in_=mBd[:, :, j * 512:(j + 1) * 512], writes=[mB[i]], stream="ldx")
        S.dma("sp", out=xo[i][:], in_=xTe_d[:, :, e0:e0 + 512], writes=[xo[i]], stream="ldx")
        seg_norm(mA[i], 6, 64, gt["g_oa"], sqA, tA, nA, 384)
        seg_norm(mB[i], 6, 64, gt["g_ob"], sqA, tA, nB, 384)
        seg_norm(yc, 2, 128, gt["g_oc"], sqC, tA, nC, 256) if False else None
        S.op("act", "activation", out=sqC[:], in_=yc[:], func=AF.Square, reads=[yc], writes=[sqC])
        for c in range(2):
            S.op("pe", "matmul", ss_ps[:], ones_bf[:], sqC[:, c, :], start=(c == 0), stop=(c == 1), reads=[ones_bf, sqC], writes=[ss_ps])
        rstd(ss_ps[:], rsn[:], 256.0, 128, ss_ps, rsn)
        for c in range(2):
            S.op("dve", "scalar_tensor_tensor", out=nC[:, c, :], in0=yc[:, c, :], scalar=gt["g_oc"][:, c:c + 1], in1=rsn[:],
                 op0=ALU.mult, op1=ALU.mult, reads=[yc, gt["g_oc"], rsn], writes=[nC])
        for dc in range(8):
            wp = w_ps[dc % 2]
            dsl = slice(dc * 128, (dc + 1) * 128)
            for h in range(6):
                S.op("pe", "matmul", wp[:], woA[:, h, dsl], nA[:, h, :], start=(h == 0), stop=False, reads=[woA, nA], writes=[wp])
            for h in range(6):
                S.op("pe", "matmul", wp[:], woB[:, h, dsl], nB[:, h, :], start=False, stop=False, reads=[woB, nB], writes=[wp])
            for c in range(2):
                S.op("pe", "matmul", wp[:], woC[:, c, dsl], nC[:, c, :], start=False, stop=(c == 1), reads=[woC, nC], writes=[wp])
            S.op("dve", "tensor_tensor", out=xm[i][:, dc, :], in0=wp[:], in1=xo[i][:, dc, :], op=ALU.add, reads=[wp, xo[i]], writes=[xm[i]])
        S.dma("pool", out=xmid_o[:, :, j * 512:(j + 1) * 512], in_=xm[i][:], reads=[xm[i]], stream="st")
        S.op("act", "activation", out=sqx[:], in_=xm[i][:], func=AF.Square, reads=[xm[i]], writes=[sqx])
        for k in range(8):
            S.op("pe", "matmul", ss_ps[:], ones_bf[:], sqx[:, k, :], start=(k == 0), stop=(k == 7), reads=[ones_bf, sqx], writes=[ss_ps])
        rstd(ss_ps[:], rsn[:], float(D), 128, ss_ps, rsn)
        S.op("dve", "tensor_tensor", out=hf[:], in0=xm[i][:], in1=gt["g_ffn"][:].unsqueeze(2).to_broadcast([128, 8, 512]), op=ALU.mult,
             reads=[xm[i], gt["g_ffn"]], writes=[hf])
        S.op("dve", "tensor_tensor", out=hf[:], in0=hf[:], in1=rsn[:].unsqueeze(1).to_broadcast([128, 8, 512]), op=ALU.mult,
             reads=[hf, rsn], writes=[hf])
        S.op("act", "activation", out=hb_[i][:], in_=hf[:], func=AF.Copy, reads=[hf], writes=[hb_[i]])
        S.dma("pool", out=hn2_o[:, :, j * 512:(j + 1) * 512], in_=hb_[i][:], reads=[hb_[i]], stream="st")
        for k in range(8):
            S.op("pe", "matmul", r_ps[0:NEXP, :], wrt[:, k, :], hf[:, k, :], start=(k == 0), stop=(k == 7), reads=[wrt, hf], writes=[r_ps])
        S.op("act", "activation", out=ex[:], in_=r_ps[0:NEXP, :], func=AF.Exp, bias=gt["b_rt"][:, 0:1], reads=[r_ps, gt["b_rt"]], writes=[ex])
        S.op("pe", "matmul", r_ps[0:NEXP, :], ones_f[0:NEXP, 0:NEXP], ex[:], start=True, stop=True, reads=[ones_f, ex], writes=[r_ps])
        S.op("dve", "reciprocal", out=rsum[:], in_=r_ps[0:NEXP, :], reads=[r_ps], writes=[rsum])
        S.op("dve", "tensor_tensor", out=affo[i][:], in0=ex[:], in1=rsum[:], op=ALU.mult, reads=[ex, rsum], writes=[affo[i]])
        S.dma("pool", out=aff_o[:, j * 512:(j + 1) * 512], in_=affo[i][:], reads=[affo[i]], stream="st")
    S.finish()
    P.close()
    G.close()
    return nc


def _chunkT(a):
    t, f = a.shape
    return np.ascontiguousarray(a.T.reshape(f // 128, 128, t).transpose(1, 0, 2))


def _rope_tables():
    half = 16
    inv = (np.float32(10000.0) ** (-(np.arange(half, dtype=np.float32)) / np.float32(half))).astype(np.float32)
    ang = np.arange(S_LEN, dtype=np.float32)[:, None] * inv[None, :]
    cos = np.cos(ang).astype(np.float32).T
    sin = np.sin(ang).astype(np.float32).T
    C = np.ones((96, S_LEN), np.float32)
    Sn = np.zeros((96, S_LEN), np.float32)
    C[64:80] = cos
    C[80:96] = cos
    Sn[64:80] = sin
    Sn[80:96] = sin
    rotT = np.zeros((96, 96), np.float32)
    for i in range(16):
        rotT[80 + i, 64 + i] = -1.0
        rotT[64 + i, 80 + i] = 1.0
    return C, Sn, rotT


def _bias_block(rpb, rb):
    qr = np.arange(8)
    r = rb + qr
    r0 = np.clip(r - 4, 0, 256 - 8)
    kr = rb - 4 + np.arange(16)
    valid_r = (kr[None, :] >= r0[:, None]) & (kr[None, :] < r0[:, None] + 8)
    row_off = np.clip(kr[None, :] - r[:, None] + 7, 0, 14)
    qc = np.arange(64)
    c0 = np.clip(qc - 8, 0, 64 - 16)
    kc = np.arange(64)
    valid_c = (kc[None, :] >= c0[:, None]) & (kc[None, :] < c0[:, None] + 16)
    col_off = np.clip(kc[None, :] - qc[:, None] + 15, 0, 30)
    g = rpb[:, row_off.T[:, None, :, None], col_off.T[None, :, None, :]]
    valid = valid_r.T[:, None, :, None] & valid_c.T[None, :, None, :]
    out = np.where(valid[None], g, np.float32(-30000.0)).astype(np.float32)
    out = out.reshape(6, 8, 128, 512).transpose(0, 2, 1, 3)
    return np.ascontiguousarray(out)


_PROGS = {}


def _prog(name):
    if name not in _PROGS:
        _PROGS[name] = build_mix() if name == "mix" else build_moe()
    return _PROGS[name]


def _mix_maps(x, l, p, consts):
    C, Sn, rotT, bd, ident = consts
    maps = []
    g1 = lambda v: np.ascontiguousarray(v.reshape(-1, 128).T)
    common = {
        "w_in": p["w_in"][l], "w_uq": p["w_uq"][l], "w_ukv": p["w_ukv"][l], "w_out": p["w_out"][l],
        "w_router": p["w_router"][l],
        "g_mix": g1(p["norm_mix"][l]),
        "g_qa": np.ascontiguousarray(np.tile(p["q_norm_a"][l], 2)[:, None]),
        "g_ka": np.ascontiguousarray(np.tile(p["k_norm_a"][l], 2)[:, None]),
        "g_cq": g1(p["cq_norm"][l]), "g_ckv": g1(p["ckv_norm"][l]),
        "g_qb": np.ascontiguousarray(p["q_norm_b"][l][:, None]), "g_kb": np.ascontiguousarray(p["k_norm_b"][l][:, None]),
        "conv_w": np.ascontiguousarray(p["conv_w"][l].T.reshape(2, 128, 3).transpose(1, 0, 2)),
        "g_oa": np.ascontiguousarray(p["out_norm"][l][0:384].reshape(6, 64).T),
        "g_ob": np.ascontiguousarray(p["out_norm"][l][384:768].reshape(6, 64).T),
        "g_oc": g1(p["out_norm"][l][768:1024]),
        "g_ffn": g1(p["norm_ffn"][l]), "b_router": np.ascontiguousarray(p["b_router"][l][:, None]),
        "ropeKC": C, "ropeKS": Sn, "rotT": rotT, "blockdiag": bd, "ident": ident,
    }
    rpb = p["rpb"][l]
    b_int = _bias_block(rpb, 64)
    xTf = [_chunkT(x[b]) for b in range(2)]
    for core in range(NCORES):
        b, q = divmod(core, 4)
        xpad = np.zeros((S_LEN + 1024, D), np.float32)
        xpad[512:512 + S_LEN] = x[b]
        m = dict(common)
        m["xT_full"] = xTf[b]
        m["xT_ext"] = _chunkT(xpad[q * TOK:q * TOK + EXT])
        m["ropeQC"] = np.ascontiguousarray(C[:, q * TOK:(q + 1) * TOK])
        m["ropeQS"] = np.ascontiguousarray(Sn[:, q * TOK:(q + 1) * TOK])
        b0 = _bias_block(rpb, 0) if q == 0 else b_int
        b2 = _bias_block(rpb, 248) if q == 3 else b_int
        m["biasA"] = np.ascontiguousarray(np.stack([b0, b_int, b2]))
        maps.append(m)
    return maps


def _run_layer(x, l, p, consts):
    res = run_bass_kernel_spmd(_prog("mix"), _mix_maps(x, l, p, consts), core_ids=list(range(NCORES))).results
    maps = []
    for core in range(NCORES):
        b, q = divmod(core, 4)
        affb = np.concatenate([res[b * 4 + i]["affT"] for i in range(4)], axis=1)
        aff_own = res[core]["affT"]
        xm = res[core]["xmidT"]
        xm_tok = xm.transpose(2, 1, 0).reshape(TOK, D)
        maps.append({
            "aff_full": np.ascontiguousarray(affb.T.reshape(128, 128 * NEXP)),
            "aff_own": np.ascontiguousarray(aff_own.T.reshape(32, 128, NEXP).transpose(1, 0, 2).reshape(128, 32 * NEXP)),
            "hn2T": res[core]["hn2T"],
            "xmid": np.ascontiguousarray(xm_tok.reshape(32, 128, D).transpose(1, 0, 2)),
            "wg": p["w_gate"][l], "wu": p["w_up"][l], "wd": p["w_down"][l],
        })
    res2 = run_bass_kernel_spmd(_prog("moe"), maps, core_ids=list(range(NCORES))).results
    xn = np.empty((2, S_LEN, D), np.float32)
    for core in range(NCORES):
        b, q = divmod(core, 4)
        xn[b, q * TOK:(q + 1) * TOK] = res2[core]["xout"].transpose(1, 0, 2).reshape(TOK, D)
    return xn, res


def kernel(**inputs):
    p = {k: np.asarray(v, dtype=np.float32) for k, v in inputs.items()}
    x = p.pop("x")
    C, Sn, rotT = _rope_tables()
    bd = np.zeros((128, 128), np.float32)
    bd[:64, :64] = 1.0
    bd[64:, 64:] = 1.0
    consts = (C, Sn, rotT, bd, np.eye(128, dtype=np.float32))
    for l in range(2):
        x, _ = _run_layer(x, l, p, consts)
    return x
```
